# Optimizing a Trainium2 kernel written in Bass

```python
import jax, jax.numpy as jnp
from jax import lax
import numpy as np

D_MODEL = 2048
BATCH = 4
SEQ = 4096
DEPTH = 1

CHUNK = 64
MIX_WIDTH = D_MODEL
CONV_CHANNELS = MIX_WIDTH // 2
CONV_WIDTH = 31
HEAD_DIM = 64
ATT_HEADS = (MIX_WIDTH - CONV_CHANNELS) // HEAD_DIM
ATT_WIDTH = ATT_HEADS * HEAD_DIM
LEFT_CHUNKS = 8
BAND = (LEFT_CHUNKS + 1) * CHUNK
MAX_REL = 256
REL_SIZE = (CHUNK - 1) + MAX_REL + 1
IN_WIDTH = 2 * CONV_CHANNELS + 3 * ATT_WIDTH
N_EXPERTS = 64
TOP_K = 8
N_GROUPS = 8
TOPK_GROUPS = 4
EXPERT_HIDDEN = 512
SHARED_HIDDEN = 512
ROUTED_SCALE = 2.5
ROW_BLOCK = 128
LN_EPS = 1e-5
ALPHA = (2 * DEPTH) ** 0.25
BETA = (8 * DEPTH) ** -0.25

kernel_name = "hybrid_conformer_chunkattn_moe_deepnorm"


def layer_norm(x, g, b):
    xf = x.astype(jnp.float32)
    mu = jnp.mean(xf, axis=-1, keepdims=True)
    var = jnp.mean(jnp.square(xf - mu), axis=-1, keepdims=True)
    return ((xf - mu) * lax.rsqrt(var + LN_EPS) * g.astype(jnp.float32) + b.astype(jnp.float32)).astype(x.dtype)


def conformer_conv_group(u, conv_w, conv_b, ln_g, ln_b):
    a, gate = jnp.split(u, 2, axis=-1)
    v = a * jax.nn.sigmoid(gate)
    y = lax.conv_general_dilated(
        v, conv_w[:, None, :].astype(v.dtype), window_strides=(1,),
        padding=[(CONV_WIDTH - 1, 0)],
        dimension_numbers=('NWC', 'WIO', 'NWC'),
        feature_group_count=CONV_CHANNELS) + conv_b
    y = layer_norm(y, ln_g, ln_b)
    return jax.nn.silu(y)


def chunked_relpos_attention(q, k, v, rel_bias):
    B, S, H, Dh = q.shape
    nc = S // CHUNK
    pad = LEFT_CHUNKS * CHUNK
    k_pad = jnp.pad(k, ((0, 0), (pad, 0), (0, 0), (0, 0)))
    v_pad = jnp.pad(v, ((0, 0), (pad, 0), (0, 0), (0, 0)))
    q_chunks = q.reshape(B, nc, CHUNK, H, Dh).transpose(1, 0, 2, 3, 4)
    rel = (pad + jnp.arange(CHUNK))[:, None] - jnp.arange(BAND)[None, :]
    idx = jnp.clip(rel, -(CHUNK - 1), MAX_REL) + (CHUNK - 1)
    bias = rel_bias[:, idx].astype(jnp.float32)
    scale = HEAD_DIM ** -0.5

    def one_chunk(args):
        qc, c = args
        start = c * CHUNK
        kc = lax.dynamic_slice_in_dim(k_pad, start, BAND, axis=1)
        vc = lax.dynamic_slice_in_dim(v_pad, start, BAND, axis=1)
        s = jnp.einsum('bqhd,bkhd->bhqk', qc, kc, preferred_element_type=jnp.float32) * scale + bias
        key_pos = start - pad + jnp.arange(BAND)
        s = jnp.where(key_pos[None, None, None, :] >= 0, s, -jnp.inf)
        p = jax.nn.softmax(s, axis=-1).astype(vc.dtype)
        return jnp.einsum('bhqk,bkhd->bqhd', p, vc)

    out = lax.map(one_chunk, (q_chunks, jnp.arange(nc)))
    return out.transpose(1, 0, 2, 3, 4).reshape(B, S, H * Dh)


def route(h, w_router, router_bias):
    n = h.shape[0]
    logits = jnp.einsum('nd,de->ne', h, w_router, preferred_element_type=jnp.float32)
    scores = jax.nn.sigmoid(logits)
    sel = scores + router_bias.astype(jnp.float32)
    grp = sel.reshape(n, N_GROUPS, N_EXPERTS // N_GROUPS)
    grp_score = jnp.sum(lax.top_k(grp, 2)[0], axis=-1)
    _, gidx = lax.top_k(grp_score, TOPK_GROUPS)
    gmask = jnp.any(gidx[..., None] == jnp.arange(N_GROUPS), axis=-2)
    emask = jnp.repeat(gmask, N_EXPERTS // N_GROUPS, axis=-1)
    _, top_idx = lax.top_k(jnp.where(emask, sel, -jnp.inf), TOP_K)
    w = jnp.take_along_axis(scores, top_idx, axis=-1)
    w = w / jnp.sum(w, axis=-1, keepdims=True) * ROUTED_SCALE
    return top_idx, w


def routed_experts(h, top_idx, gate, w_gate, w_up, w_down):
    n, d = h.shape
    nk = n * TOP_K
    flat_e = top_idx.reshape(nk)
    flat_tok = jnp.arange(nk, dtype=jnp.int32) // TOP_K
    flat_w = gate.reshape(nk)
    order = jnp.argsort(flat_e)
    e_sorted = flat_e[order]
    counts = jnp.bincount(flat_e, length=N_EXPERTS)
    starts = jnp.cumsum(counts) - counts
    padded = (counts + ROW_BLOCK - 1) // ROW_BLOCK * ROW_BLOCK
    pad_ends = jnp.cumsum(padded)
    pad_starts = pad_ends - padded
    dest = pad_starts[e_sorted] + jnp.arange(nk) - starts[e_sorted]
    n_blocks = -(-nk // ROW_BLOCK) + N_EXPERTS
    n_rows = n_blocks * ROW_BLOCK
    row_tok = jnp.full((n_rows,), n, jnp.int32).at[dest].set(flat_tok[order])
    row_w = jnp.zeros((n_rows,), jnp.float32).at[dest].set(flat_w[order])
    block_e = jnp.minimum(
        jnp.searchsorted(pad_ends, jnp.arange(n_blocks) * ROW_BLOCK, side='right'), N_EXPERTS - 1)
    h_pad = jnp.concatenate([h, jnp.zeros((1, d), h.dtype)], axis=0)

    def one_block(args):
        tok, wts, e = args
        xb = h_pad[tok]
        hid = jax.nn.silu(xb @ w_gate[e]) * (xb @ w_up[e])
        return (hid @ w_down[e]) * wts[:, None].astype(h.dtype)

    y = lax.map(one_block, (row_tok.reshape(n_blocks, ROW_BLOCK),
                            row_w.reshape(n_blocks, ROW_BLOCK), block_e))
    y = y.reshape(n_rows, d)
    return jax.ops.segment_sum(y, row_tok, num_segments=n + 1)[:n]


def setup_inputs(seed: int = 0) -> dict:
    key = jax.random.key(seed)
    ks = jax.random.split(key, 24)
    f32 = jnp.float32
    nrm = lambda k, shape, s: (jax.random.normal(k, shape, f32) * s).astype(f32)
    return {
        "x": nrm(ks[0], (BATCH, SEQ, D_MODEL), 1.0),
        "ln_in_g": 1.0 + nrm(ks[1], (D_MODEL,), 0.05),
        "ln_in_b": nrm(ks[2], (D_MODEL,), 0.02),
        "w_in": nrm(ks[3], (DEPTH, D_MODEL, IN_WIDTH), D_MODEL ** -0.5),
        "conv_w": nrm(ks[4], (DEPTH, CONV_WIDTH, CONV_CHANNELS), CONV_WIDTH ** -0.5),
        "conv_b": nrm(ks[5], (DEPTH, CONV_CHANNELS), 0.02),
        "conv_ln_g": 1.0 + nrm(ks[6], (DEPTH, CONV_CHANNELS), 0.05),
        "conv_ln_b": nrm(ks[7], (DEPTH, CONV_CHANNELS), 0.02),
        "rel_bias": nrm(ks[8], (DEPTH, ATT_HEADS, REL_SIZE), 0.2),
        "w_out": nrm(ks[9], (DEPTH, MIX_WIDTH, D_MODEL), MIX_WIDTH ** -0.5 * BETA),
        "ln1_g": 1.0 + nrm(ks[10], (DEPTH, D_MODEL), 0.05),
        "ln1_b": nrm(ks[11], (DEPTH, D_MODEL), 0.02),
        "w_router": nrm(ks[12], (DEPTH, D_MODEL, N_EXPERTS), D_MODEL ** -0.5),
        "router_bias": nrm(ks[13], (DEPTH, N_EXPERTS), 0.01),
        "w_gate": nrm(ks[14], (DEPTH, N_EXPERTS, D_MODEL, EXPERT_HIDDEN), D_MODEL ** -0.5),
        "w_up": nrm(ks[15], (DEPTH, N_EXPERTS, D_MODEL, EXPERT_HIDDEN), D_MODEL ** -0.5),
        "w_down": nrm(ks[16], (DEPTH, N_EXPERTS, EXPERT_HIDDEN, D_MODEL), EXPERT_HIDDEN ** -0.5 * BETA),
        "w_shared_gate": nrm(ks[17], (DEPTH, D_MODEL, SHARED_HIDDEN), D_MODEL ** -0.5),
        "w_shared_up": nrm(ks[18], (DEPTH, D_MODEL, SHARED_HIDDEN), D_MODEL ** -0.5),
        "w_shared_down": nrm(ks[19], (DEPTH, SHARED_HIDDEN, D_MODEL), SHARED_HIDDEN ** -0.5 * BETA),
        "ln2_g": 1.0 + nrm(ks[20], (DEPTH, D_MODEL), 0.05),
        "ln2_b": nrm(ks[21], (DEPTH, D_MODEL), 0.02),
    }


def reference(x, ln_in_g, ln_in_b, w_in, conv_w, conv_b, conv_ln_g, conv_ln_b, rel_bias,
              w_out, ln1_g, ln1_b, w_router, router_bias, w_gate, w_up, w_down,
              w_shared_gate, w_shared_up, w_shared_down, ln2_g, ln2_b):
    B, S, D = x.shape
    h = layer_norm(x, ln_in_g, ln_in_b)
    c2 = 2 * CONV_CHANNELS
    for i in range(DEPTH):
        u = jnp.einsum('bsd,de->bse', h, w_in[i])
        u_conv, q, k, v = jnp.split(u, [c2, c2 + ATT_WIDTH, c2 + 2 * ATT_WIDTH], axis=-1)
        conv_out = conformer_conv_group(u_conv, conv_w[i], conv_b[i], conv_ln_g[i], conv_ln_b[i])
        att_out = chunked_relpos_attention(
            q.reshape(B, S, ATT_HEADS, HEAD_DIM), k.reshape(B, S, ATT_HEADS, HEAD_DIM),
            v.reshape(B, S, ATT_HEADS, HEAD_DIM), rel_bias[i])
        mixed = jnp.einsum('bse,ed->bsd', jnp.concatenate([conv_out, att_out], axis=-1), w_out[i])
        h = layer_norm(ALPHA * h + mixed, ln1_g[i], ln1_b[i])
        hf = h.reshape(B * S, D)
        top_idx, gate = route(hf, w_router[i], router_bias[i])
        y = routed_experts(hf, top_idx, gate, w_gate[i], w_up[i], w_down[i])
        y = y + (jax.nn.silu(hf @ w_shared_gate[i]) * (hf @ w_shared_up[i])) @ w_shared_down[i]
        h = layer_norm(ALPHA * h + y.reshape(B, S, D), ln2_g[i], ln2_b[i])
    return h
```

```python
import numpy as np
from contextlib import ExitStack
import concourse.bass as bass
import concourse.mybir as mybir
from concourse.bass_utils import run_bass_kernel_spmd

F32, BF16, I32 = mybir.dt.float32, mybir.dt.bfloat16, mybir.dt.int32
AF = mybir.ActivationFunctionType
ALU = mybir.AluOpType

D = 2048
NT = 2048
HALO = 512
NTH = NT + HALO
NE = 64
CAP = 512
CAPV = 448
NTL = CAP // 128
NS = NE * CAP
FH = 512
ALPHA = 2.0 ** 0.25
EPS = 1e-5
NCONST = 640


class Tok:
    __slots__ = ("sem", "val")

    def __init__(self, sem, val):
        self.sem = sem
        self.val = val


def _flat(ts):
    for t in ts:
        if t is None:
            continue
        if isinstance(t, (list, tuple)):
            yield from _flat(t)
        else:
            yield t


class Eng:
    def __init__(self, nc, h, name, st):
        self.h = h
        self.sem = st.enter_context(nc.semaphore("s_" + name))
        self.n = 0
        self.seen = {}
        self.last = None

    def wait(self, *toks):
        for t in _flat(toks):
            k = id(t.sem)
            if self.seen.get(k, 0) < t.val:
                self.h.wait_ge(t.sem, t.val)
                self.seen[k] = t.val

    def done(self, ins):
        self.n += 1
        ins.then_inc(self.sem, 1)
        self.last = Tok(self.sem, self.n)
        return self.last

    def op(self, fn, *deps):
        self.wait(*deps)
        return self.done(fn())

    def raw(self, fn, *deps):
        self.wait(*deps)
        fn()


class Slot:
    allslots = []

    def __init__(self, nc, name, st):
        self.sem = st.enter_context(nc.semaphore("d_" + name))
        self.cnt = 0
        Slot.allslots.append(self)

    def dma(self, q, fn, *deps):
        q.wait(*deps)
        self.cnt += 16
        fn().then_inc(self.sem, 16)
        return Tok(self.sem, self.cnt)

    def tok(self):
        return Tok(self.sem, self.cnt) if self.cnt else None


def build_nc(stage=99):
    Slot.allslots = []
    nc = bass.Bass("TRN2", target_bir_lowering=False)

    def din(name, shape, dt=F32):
        return nc.dram_tensor(name, shape, dt, kind="ExternalInput").ap()

    x = din("x", [NTH, D])
    flags = din("flags", [128, 2])
    consts = din("consts", [128, NCONST])
    ln_in_gb = din("ln_in_gb", [2, D])
    w_in = din("w_in", [D, 5120])
    conv_wT = din("conv_wT", [128, 8 * 31])
    conv_par = din("conv_par", [128, 24])
    biasT = din("biasT", [128, 16 * 5 * 128])
    w_out = din("w_out", [D, D])
    ln12 = din("ln12", [4, D])
    w_router = din("w_router", [D, NE])
    router_bias = din("router_bias", [1, NE])
    if stage >= 5:
        w_gate = din("w_gate", [NE, D, FH])
        w_up = din("w_up", [NE, D, FH])
        w_down = din("w_down", [NE, FH, D])
    wsg = din("wsg", [D, FH])
    wsu = din("wsu", [D, FH])
    wsd = din("wsd", [FH, D])
    y = nc.dram_tensor("y", [NT, D], F32, kind="ExternalOutput").ap()

    def dscr(name, shape, dt, dbg=False):
        return nc.dram_tensor(name, shape, dt, kind="ExternalOutput" if dbg else "Internal").ap()

    hbuf = dscr("hbuf", [NT, D], F32)
    h1buf = dscr("h1buf", [NT, D], F32, dbg=(stage == 2))
    h1bf = dscr("h1bf", [NT, D], BF16)
    yshbuf = dscr("yshbuf", [NT, D], BF16, dbg=(stage == 3))
    slot_tok = dscr("slot_tok", [NS + 128, 4], I32, dbg=(stage == 4))
    ybuf = dscr("ybuf", [NS + 128, D], BF16)
    dbg_cat = dscr("dbg_cat", [128, 16 * NT], BF16, dbg=(stage == 1)) if stage == 1 else None
    dbg_gate = dscr("dbg_gate", [128, 16 * 64], F32, dbg=True) if stage in (3, 4) else None
    dbg_s8 = dscr("dbg_s8", [128, 16 * 8], I32, dbg=True) if stage == 4 else None

    with ExitStack() as st:
        PE = Eng(nc, nc.tensor, "pe", st)
        ACT = Eng(nc, nc.scalar, "act", st)
        DVE = Eng(nc, nc.vector, "dve", st)
        POOL = Eng(nc, nc.gpsimd, "pool", st)
        SP = Eng(nc, nc.sync, "sp", st)
        engines = [PE, ACT, DVE, POOL, SP]

        def sb(name, shape, dt, stack=st):
            return stack.enter_context(nc.sbuf_tensor(name, shape, dt))

        def pst(name, shape, dt, stack):
            return stack.enter_context(nc.psum_tensor(name, shape, dt))

        def barrier(exclude=()):
            toks = [e.last for e in engines] + [s.tok() for s in Slot.allslots if s not in exclude]
            for e in engines:
                e.wait(toks)

        cst = sb("cst", [128, NCONST], F32)
        ident_bf = sb("ident_bf", [128, 128], BF16)
        lstr_bf = sb("lstr_bf", [128, 128], BF16)
        ones_bf = sb("ones_bf", [128, 128], BF16)
        flg = sb("flg", [128, 2], F32)
        eps_t = sb("eps_t", [128, 1], F32)
        ph_cat = ExitStack()
        catT = sb("catT", [128, 16, NT], BF16, ph_cat)
        s_c = Slot(nc, "c", st)
        t_c = s_c.dma(SP, lambda: nc.sync.dma_start(out=cst[:], in_=consts[:, :]))
        t_c = s_c.dma(SP, lambda: nc.sync.dma_start(out=flg[:], in_=flags[:, :]))
        t0 = DVE.op(lambda: nc.vector.tensor_copy(out=ident_bf[:], in_=cst[:, 0:128]), t_c)
        t0 = DVE.op(lambda: nc.vector.tensor_copy(out=lstr_bf[:], in_=cst[:, 128:256]))
        t0 = DVE.op(lambda: nc.vector.tensor_copy(out=ones_bf[:], in_=cst[:, 256:384]))
        t_const = DVE.op(lambda: nc.vector.memset(eps_t[:], EPS))
        ones_f = cst[:, 256:384]
        hv = flg[:, 0:1]

        def ln_stats(xt, n, tok_in, pool):
            stats, mv, sd = pool
            DVE.wait(tok_in)
            nchunk = n // 512
            for j in range(nchunk):
                t = DVE.op(lambda j=j: nc.vector.bn_stats(out=stats[:, j * 6:(j + 1) * 6], in_=xt[:, j * 512:(j + 1) * 512]))
            t = DVE.op(lambda: nc.vector.bn_aggr(out=mv[:, 0:2], in_=stats[:, 0:6 * nchunk]), t)
            t = ACT.op(lambda: nc.scalar.activation(out=sd[:, 0:1], in_=mv[:, 1:2], func=AF.Sqrt, bias=eps_t[:, 0:1], scale=1.0), t, t_const)
            t = DVE.op(lambda: nc.vector.reciprocal(out=sd[:, 1:2], in_=sd[:, 0:1]), t)
            return mv[:, 0:1], sd[:, 1:2], t

        def ln_norm_act(buf, tok_in, pool):
            mean, rstd, t = ln_stats(buf, D, tok_in, pool)
            sd = pool[2]
            t = DVE.op(lambda: nc.vector.tensor_scalar(out=sd[:, 2:3], in0=mean, scalar1=rstd, scalar2=-1.0, op0=ALU.mult, op1=ALU.mult), t)
            return ACT.op(lambda: nc.scalar.activation(out=buf[:], in_=buf[:], func=AF.Identity, bias=sd[:, 2:3], scale=rstd), t)

        with ExitStack() as ph_mix:
            hT = sb("hT", [128, 16, NTH], BF16, ph_mix)
            with ExitStack() as ph:
                gbrow = sb("gbrow", [128, 2, D], F32, ph)
                xt = [sb(f"xt{i}", [128, D], F32, ph) for i in range(2)]
                hf = [sb(f"hf{i}", [128, D], F32, ph) for i in range(2)]
                hb = [sb(f"hb{i}", [128, D], BF16, ph) for i in range(2)]
                lnp = [(sb(f"st{i}", [128, 24], F32, ph), sb(f"mv{i}", [128, 2], F32, ph), sb(f"sd{i}", [128, 2], F32, ph)) for i in range(2)]
                ptr = [pst(f"ptr{i}", [128, 1024], BF16, ph) for i in range(4)]
                s_x = [Slot(nc, f"x{i}", st) for i in range(2)]
                s_h = [Slot(nc, f"h{i}", st) for i in range(2)]
                s_g = Slot(nc, "gb", st)
                t_g = s_g.dma(SP, lambda: nc.sync.dma_start(out=gbrow[:, 0, :], in_=ln_in_gb[0:1, :].partition_broadcast(128)))
                t_g = s_g.dma(SP, lambda: nc.sync.dma_start(out=gbrow[:, 1, :], in_=ln_in_gb[1:2, :].partition_broadcast(128)))
                x_free = [None, None]
                hf_free = [None, None]
                hb_free = [None, None]
                ptr_free = [None] * 4
                NTI = NTH // 128
                t_hfs = [None] * NTI
                for it in range(NTI + 1):
                    if it < NTI:
                        tt = it
                        s = tt % 2
                        t_ld = s_x[s].dma(SP, lambda: nc.sync.dma_start(out=xt[s][:], in_=x[tt * 128:(tt + 1) * 128, :]), x_free[s])
                        mean, rstd, t = ln_stats(xt[s], D, t_ld, lnp[s])
                        t = DVE.op(lambda: nc.vector.tensor_scalar(out=hf[s][:], in0=xt[s][:], scalar1=mean, scalar2=rstd, op0=ALU.subtract, op1=ALU.mult), t, hf_free[s])
                        x_free[s] = t
                        t = DVE.op(lambda: nc.vector.tensor_tensor(out=hf[s][:], in0=hf[s][:], in1=gbrow[:, 0, :], op=ALU.mult), t, t_g)
                        t_hfs[tt] = POOL.op(lambda: nc.gpsimd.tensor_tensor(out=hf[s][:], in0=hf[s][:], in1=gbrow[:, 1, :], op=ALU.add), t)
                    if it >= 1:
                        tt = it - 1
                        s = tt % 2
                        t_hf = t_hfs[tt]
                        t_hb = ACT.op(lambda: nc.scalar.copy(out=hb[s][:], in_=hf[s][:]), t_hf, hb_free[s])
                        if tt >= HALO // 128:
                            r0 = (tt - HALO // 128) * 128
                            t_st = s_h[s].dma(SP, lambda: nc.sync.dma_start(out=hbuf[r0:r0 + 128, :], in_=hf[s][:]), t_hf)
                            hf_free[s] = [t_hb, t_st]
                        else:
                            hf_free[s] = t_hb
                        tps = []
                        for g in range(2):
                            pi = (tt % 2) * 2 + g
                            PE.wait(t_hb, ptr_free[pi])
                            for j in range(8):
                                dc = g * 8 + j
                                ins = nc.tensor.transpose(out=ptr[pi][:, j * 128:(j + 1) * 128], in_=hb[s][:, dc * 128:(dc + 1) * 128], identity=ident_bf[:])
                            tp = PE.done(ins)
                            if g == 0:
                                te = ACT.op(lambda: nc.scalar.copy(out=hT[:, 0:8, tt * 128:(tt + 1) * 128], in_=ptr[pi][:, :].rearrange("p (a b) -> p a b", a=8)), tp)
                            else:
                                te = DVE.op(lambda: nc.vector.tensor_copy(out=hT[:, 8:16, tt * 128:(tt + 1) * 128], in_=ptr[pi][:, :].rearrange("p (a b) -> p a b", a=8)), tp)
                            ptr_free[pi] = te
                            tps.append(tp)
                        hb_free[s] = tps
                barrier()

            with ExitStack() as ph:
                wc = [sb(f"wc{i}", [128, 16, 256], BF16, ph) for i in range(2)]
                dg = [sb(f"dg{i}", [128, 31, 128], BF16, ph) for i in range(2)]
                vT = [sb(f"vT{i}", [128, 32 + NT], BF16, ph) for i in range(2)]
                cw = sb("cw", [128, 8 * 31], F32, ph)
                cpar = sb("cpar", [128, 24], F32, ph)
                sig = [sb(f"sig{i}", [128, 512], F32, ph) for i in range(2)]
                pA = [pst(f"pA{i}", [128, 512], F32, ph) for i in range(2)]
                pG = [pst(f"pG{i}", [128, 512], F32, ph) for i in range(2)]
                pY = [pst(f"pY{i}", [128, 512], F32, ph) for i in range(2)]
                pS = [pst(f"pS{i}", [128, 512], F32, ph) for i in range(2)]
                s_w = [Slot(nc, f"wc{i}", st) for i in range(2)]
                s_p = Slot(nc, "cpar", st)
                t_p = s_p.dma(SP, lambda: nc.sync.dma_start(out=cw[:], in_=conv_wT[:, :]))
                t_p = s_p.dma(SP, lambda: nc.sync.dma_start(out=cpar[:], in_=conv_par[:, :]))
                w_in_v = w_in.rearrange("(c p) n -> p c n", p=128)
                wc_free = [None, None]
                dg_free = [None, None]
                vT_free = [None, None]
                pA_free = [None, None]
                pG_free = [None, None]
                pY_free = [None, None]
                sig_free = [None, None]
                ia = 0
                iy = 0
                for ct in range(8):
                    s = ct % 2
                    t_w = s_w[s].dma(POOL, lambda: nc.gpsimd.dma_start(out=wc[s][:, :, 0:128], in_=w_in_v[:, :, ct * 128:(ct + 1) * 128]), wc_free[s])
                    t_w = s_w[s].dma(POOL, lambda: nc.gpsimd.dma_start(out=wc[s][:, :, 128:256], in_=w_in_v[:, :, 1024 + ct * 128:1024 + (ct + 1) * 128]))
                    POOL.wait(t_p, dg_free[s], t_const)
                    for k in range(31):
                        t_dg = POOL.op(lambda k=k: nc.gpsimd.tensor_scalar(out=dg[s][:, k, :], in0=ident_bf[:], scalar1=cw[:, ct * 31 + k:ct * 31 + k + 1], scalar2=1.0, op0=ALU.mult, op1=ALU.mult))
                    t_v_prev = None
                    last_pe = None
                    for tb in range(-1, 4):
                        if tb < 0:
                            n, c0 = 32, HALO - 32
                        else:
                            n, c0 = 512, HALO + tb * 512
                        a = ia % 2
                        ia += 1
                        PE.wait(t_w, pA_free[a])
                        for dc in range(16):
                            ins = nc.tensor.matmul(pA[a][:, 0:n], lhsT=wc[s][:, dc, 0:128], rhs=hT[:, dc, c0:c0 + n], start=(dc == 0), stop=(dc == 15))
                        tA = PE.done(ins)
                        PE.wait(pG_free[a])
                        for dc in range(16):
                            ins = nc.tensor.matmul(pG[a][:, 0:n], lhsT=wc[s][:, dc, 128:256], rhs=hT[:, dc, c0:c0 + n], start=(dc == 0), stop=(dc == 15))
                        tG = PE.done(ins)
                        t_sig = ACT.op(lambda: nc.scalar.activation(out=sig[a][:, 0:n], in_=pG[a][:, 0:n], func=AF.Sigmoid), tG, sig_free[a])
                        pG_free[a] = t_sig
                        if tb < 0:
                            t_v = DVE.op(lambda: nc.vector.scalar_tensor_tensor(out=vT[s][:, 0:32], in0=pA[a][:, 0:32], scalar=hv, in1=sig[a][:, 0:32], op0=ALU.mult, op1=ALU.mult), tA, t_sig, vT_free[s])
                        else:
                            t_v = DVE.op(lambda: nc.vector.tensor_tensor(out=vT[s][:, 32 + tb * 512:32 + (tb + 1) * 512], in0=pA[a][:, :], in1=sig[a][:, :], op=ALU.mult), tA, t_sig, vT_free[s])
                        pA_free[a] = t_v
                        sig_free[a] = t_v
                        if tb >= 0:
                            yb = iy % 2
                            iy += 1
                            PE.wait(t_v, t_v_prev, t_dg, pY_free[yb])
                            for k in range(31):
                                ins = nc.tensor.matmul(pY[yb][:, :], lhsT=dg[s][:, k, :], rhs=vT[s][:, 2 + tb * 512 + k:2 + tb * 512 + k + 512], start=(k == 0), stop=(k == 30))
                            tY = PE.done(ins)
                            last_pe = tY
                            t_e = ACT.op(lambda: nc.scalar.activation(out=catT[:, ct, tb * 512:(tb + 1) * 512], in_=pY[yb][:, :], func=AF.Identity, bias=cpar[:, ct:ct + 1], scale=1.0), tY, t_p)
                            pY_free[yb] = t_e
                        t_v_prev = t_v
                    wc_free[s] = last_pe
                    dg_free[s] = last_pe
                    vT_free[s] = last_pe
                barrier()
                sq = [sb(f"sq{i}", [128, 512], F32, ph) for i in range(2)]
                mean_s = sb("mean_s", [128, 512], F32, ph)
                rstd_s = sb("rstd_s", [128, 512], F32, ph)
                tmpv = sb("tmpv", [128, 512], F32, ph)
                zt = [sb(f"zt{i}", [128, 512], F32, ph) for i in range(2)]
                sq_free = [None, None]
                zt_free = [None, None]
                t_prev_norm = None
                for tb in range(4):
                    cs = slice(tb * 512, (tb + 1) * 512)
                    PE.wait(t_prev_norm)
                    for ct in range(8):
                        ins = nc.tensor.matmul(pS[0][:, :], lhsT=ones_bf[:], rhs=catT[:, ct, cs], start=(ct == 0), stop=(ct == 7))
                    tS1 = PE.done(ins)
                    for ct in range(8):
                        q = ct % 2
                        t_sq = DVE.op(lambda: nc.vector.tensor_tensor(out=sq[q][:], in0=catT[:, ct, cs], in1=catT[:, ct, cs], op=ALU.mult), sq_free[q])
                        PE.wait(t_sq)
                        ins = nc.tensor.matmul(pS[1][:, :], lhsT=ones_f, rhs=sq[q][:], start=(ct == 0), stop=(ct == 7))
                        sq_free[q] = PE.done(ins)
                    tS2 = sq_free[1]
                    t = DVE.op(lambda: nc.vector.tensor_scalar(out=mean_s[:], in0=pS[0][:, :], scalar1=1.0 / 1024, scalar2=None, op0=ALU.mult), tS1, t_prev_norm)
                    t = DVE.op(lambda: nc.vector.tensor_tensor(out=tmpv[:], in0=mean_s[:], in1=mean_s[:], op=ALU.mult), t)
                    t = DVE.op(lambda: nc.vector.scalar_tensor_tensor(out=tmpv[:], in0=pS[1][:, :], scalar=1.0 / 1024, in1=tmpv[:], op0=ALU.mult, op1=ALU.subtract), t, tS2)
                    t = ACT.op(lambda: nc.scalar.activation(out=tmpv[:], in_=tmpv[:], func=AF.Sqrt, bias=eps_t[:, 0:1], scale=1.0), t)
                    t_r = DVE.op(lambda: nc.vector.reciprocal(out=rstd_s[:], in_=tmpv[:]), t)
                    for ct in range(8):
                        q = ct % 2
                        t = DVE.op(lambda: nc.vector.tensor_tensor(out=zt[q][:], in0=catT[:, ct, cs], in1=mean_s[:], op=ALU.subtract), t_r, zt_free[q])
                        t = DVE.op(lambda: nc.vector.tensor_tensor(out=zt[q][:], in0=zt[q][:], in1=rstd_s[:], op=ALU.mult), t)
                        t = ACT.op(lambda: nc.scalar.activation(out=catT[:, ct, cs], in_=zt[q][:], func=AF.Silu, bias=cpar[:, 16 + ct:17 + ct], scale=cpar[:, 8 + ct:9 + ct]), t)
                        zt_free[q] = t
                    t_prev_norm = t
                barrier()

            with ExitStack() as ph:
                bT = [sb(f"bT{i}", [128, 1280], BF16, ph) for i in range(2)]
                wq = [sb(f"wq{i}", [128, 16, 384], BF16, ph) for i in range(2)]
                kT = [sb(f"kT{i}", [128, NTH], BF16, ph) for i in range(1)] * 2
                qTm = [sb(f"qTm{i}", [128, 2, NT], BF16, ph) for i in range(1)] * 2
                va = [sb(f"va{i}", [128, 20, 2, 65], BF16, ph) for i in range(1)] * 2
                pT = [sb(f"pT{i}", [128, 5, 128], BF16, ph) for i in range(2)]
                att = [sb(f"att{i}", [128, 128], BF16, ph) for i in range(2)]
                rinv = [sb(f"rinv{i}", [128, 1], F32, ph) for i in range(2)]

                pSc = [pst(f"pSc{i}", [128, 1024], F32, ph) for i in range(2)]
                pO = [pst(f"pO{i}", [128, 512], F32, ph) for i in range(2)]
                pP = [pst(f"pP{i}", [128, 512], F32, ph) for i in range(2)]
                pTr = pP[1][:, :].bitcast(BF16)
                s_b = [Slot(nc, f"bT{i}", st) for i in range(2)]
                s_wq = [Slot(nc, f"wq{i}", st) for i in range(2)]
                t_init = None
                for i in range(1):
                    t_init = POOL.op(lambda: nc.gpsimd.memset(qTm[i][64:128, 0, :], 0.0))
                    t_init = POOL.op(lambda: nc.gpsimd.memset(qTm[i][0:64, 1, :], 0.0))
                    t_init = POOL.op(lambda: nc.gpsimd.memset(va[i][:, 4:20, :, 64:65], 1.0))
                    t_init = POOL.op(lambda: nc.gpsimd.tensor_scalar(out=va[i][:, 0:4, :, 64:65], in0=va[i][:, 4:8, :, 64:65], scalar1=hv, scalar2=1.0, op0=ALU.mult, op1=ALU.mult), t_init)
                wq_free = [None, None]
                pP_free = [None, None]
                pSc_free = [None, None]
                pT_free = [None, None]
                pO_free = [None, None]
                att_free = [None, None]
                pTr_free = None
                ip = 0
                isc = 0
                iatt = 0
                prev_att = None
                for hp in range(8):
                    s = hp % 2
                    t_b = s_b[s].dma(POOL, lambda: nc.gpsimd.dma_start(out=bT[s][:, :], in_=biasT[:, hp * 1280:(hp + 1) * 1280]), wq_free[s])
                    for j in range(3):
                        c0 = 2048 + j * 1024 + hp * 128
                        t_w = s_wq[s].dma(POOL, lambda: nc.gpsimd.dma_start(out=wq[s][:, :, j * 128:(j + 1) * 128], in_=w_in_v[:, :, c0:c0 + 128]), wq_free[s] if j == 0 else None)
                    for blk in range(5):
                        p = ip % 2
                        ip += 1
                        PE.wait(t_w, pP_free[p])
                        for dc in range(16):
                            ins = nc.tensor.matmul(pP[p][:, :], lhsT=wq[s][:, dc, 128:256], rhs=hT[:, dc, blk * 512:(blk + 1) * 512], start=(dc == 0), stop=(dc == 15))
                        tP = PE.done(ins)
                        pP_free[p] = ACT.op(lambda: nc.scalar.copy(out=kT[s][:, blk * 512:(blk + 1) * 512], in_=pP[p][:, :]), tP, prev_att)
                    t_k = ACT.last
                    for blk in range(4):
                        p = ip % 2
                        ip += 1
                        PE.wait(pP_free[p])
                        for dc in range(16):
                            ins = nc.tensor.matmul(pP[p][:, :], lhsT=wq[s][:, dc, 0:128], rhs=hT[:, dc, HALO + blk * 512:HALO + (blk + 1) * 512], start=(dc == 0), stop=(dc == 15))
                        tP = PE.done(ins)
                        t1 = DVE.op(lambda: nc.vector.tensor_scalar(out=qTm[s][0:64, 0, blk * 512:(blk + 1) * 512], in0=pP[p][0:64, :], scalar1=0.125, scalar2=None, op0=ALU.mult), tP, prev_att, t_init)
                        t2 = DVE.op(lambda: nc.vector.tensor_scalar(out=qTm[s][64:128, 1, blk * 512:(blk + 1) * 512], in0=pP[p][64:128, :], scalar1=0.125, scalar2=None, op0=ALU.mult))
                        pP_free[p] = t2
                    t_q = t2
                    for g4 in range(5):
                        p = ip % 2
                        ip += 1
                        PE.wait(pP_free[p])
                        for j in range(4):
                            tile_i = g4 * 4 + j
                            for dc in range(16):
                                ins = nc.tensor.matmul(pP[p][:, j * 128:(j + 1) * 128], lhsT=hT[:, dc, tile_i * 128:(tile_i + 1) * 128], rhs=wq[s][:, dc, 256:384], start=(dc == 0), stop=(dc == 15))
                        tP = PE.done(ins)
                        src = pP[p][:, :].rearrange("p (a b c) -> p a b c", a=4, b=2)
                        if g4 == 0:
                            t_v = DVE.op(lambda: nc.vector.tensor_scalar(out=va[s][:, 0:4, :, 0:64], in0=src, scalar1=hv, scalar2=None, op0=ALU.mult), tP, prev_att, t_init)
                        else:
                            t_v = ACT.op(lambda: nc.scalar.copy(out=va[s][:, g4 * 4:(g4 + 1) * 4, :, 0:64], in_=src), tP, prev_att, t_init)
                        pP_free[p] = t_v
                    t_vd = DVE.last
                    t_va = ACT.last
                    last_pe = None
                    items = [(cp, h) for cp in range(16) for h in range(2)]

                    def emit_S(i):
                        cp, h = items[i]
                        b = i % 2
                        PE.wait(t_b, t_k, t_q, pSc_free[b], t_const)
                        for blk in range(5):
                            o = pSc[b][:, blk * 128:(blk + 1) * 128]
                            nc.tensor.matmul(o, lhsT=ident_bf[:], rhs=bT[s][:, (h * 5 + blk) * 128:(h * 5 + blk + 1) * 128], start=True, stop=False)
                            ins = nc.tensor.matmul(o, lhsT=kT[s][:, (cp + blk) * 128:(cp + blk + 1) * 128], rhs=qTm[s][:, h, cp * 128:(cp + 1) * 128], start=False, stop=True)
                        tS = PE.done(ins)
                        ACT.op(lambda: nc.scalar.activation(out=pT[b][:, 0:4, :], in_=pSc[b][:, 0:512].rearrange("p (a b) -> p a b", a=4), func=AF.Exp), tS, pT_free[b])
                        t_e2 = ACT.op(lambda: nc.scalar.activation(out=pT[b][:, 4, :], in_=pSc[b][:, 512:640], func=AF.Exp))
                        pSc_free[b] = t_e2
                        return t_e2

                    t_es = {0: emit_S(0)}
                    for i in range(32):
                        cp, h = items[i]
                        b = i % 2
                        ai = cp % 2
                        if i + 1 < 32:
                            t_es[i + 1] = emit_S(i + 1)
                        PE.wait(t_es[i], t_vd, t_va, pO_free[b])
                        for blk in range(5):
                            ins = nc.tensor.matmul(pO[b][:, 0:65], lhsT=pT[b][:, blk, :], rhs=va[s][:, cp + blk, h, :], start=(blk == 0), stop=(blk == 4))
                        tO = PE.done(ins)
                        pT_free[b] = tO
                        t_r = DVE.op(lambda: nc.vector.reciprocal(out=rinv[b][:], in_=pO[b][:, 64:65]), tO)
                        t_a = DVE.op(lambda: nc.vector.tensor_scalar(out=att[ai][:, h * 64:(h + 1) * 64], in0=pO[b][:, 0:64], scalar1=rinv[b][:, 0:1], scalar2=None, op0=ALU.mult), t_r, att_free[ai])
                        pO_free[b] = t_a
                        if h == 1:
                            PE.wait(t_a, pTr_free, pP_free[1])
                            tT = PE.done(nc.tensor.transpose(out=pTr[:, 0:128], in_=att[ai][:], identity=ident_bf[:]))
                            att_free[ai] = tT
                            last_pe = tT
                            pTr_free = ACT.op(lambda: nc.scalar.copy(out=catT[:, 8 + hp, cp * 128:(cp + 1) * 128], in_=pTr[:, 0:128]), tT)
                    wq_free[s] = last_pe
                    prev_att = last_pe
                    pP_free[1] = [pP_free[1], pTr_free]
                barrier()
        if stage == 1:
            s_d = Slot(nc, "dbg", st)
            t = s_d.dma(SP, lambda: nc.sync.dma_start(out=dbg_cat[:, :], in_=catT[:].rearrange("p a b -> p (a b)")))
            SP.wait(t)
            ph_cat.close()
            return nc


        with ExitStack() as ph:
            wo = sb("wo", [128, 16, D], BF16, ph)
            l1 = sb("l1", [128, 2, D], F32, ph)
            hld = [sb(f"hld{i}", [128, D], F32, ph) for i in range(3)]
            h1f = [sb(f"h1f{i}", [128, D], F32, ph) for i in range(2)]
            h1b = [sb(f"h1b{i}", [128, D], BF16, ph) for i in range(2)]
            lnp = [(sb(f"st4{i}", [128, 24], F32, ph), sb(f"mv4{i}", [128, 2], F32, ph), sb(f"sd4{i}", [128, 4], F32, ph)) for i in range(3)]
            pM = [pst(f"pM{i}", [128, 512], F32, ph) for i in range(8)]
            s_wo = Slot(nc, "wo", st)
            s_l1 = Slot(nc, "l1", st)
            s_hl = [Slot(nc, f"hl{i}", st) for i in range(3)]
            s_o1 = [Slot(nc, f"o1{i}", st) for i in range(2)]
            s_o2 = [Slot(nc, f"o2{i}", st) for i in range(2)]
            for dc in range(16):
                t_wo = s_wo.dma(POOL, lambda: nc.gpsimd.dma_start(out=wo[:, dc, :], in_=w_out[dc * 128:(dc + 1) * 128, :]))
            t_l1 = s_l1.dma(SP, lambda: nc.sync.dma_start(out=l1[:, 0, :], in_=ln12[0:1, :].partition_broadcast(128)))
            t_l1 = s_l1.dma(SP, lambda: nc.sync.dma_start(out=l1[:, 1, :], in_=ln12[1:2, :].partition_broadcast(128)))
            hld_free = [None] * 3
            h1f_free = [None, None]
            h1b_free = [None, None]
            pM_free = [None, None]

            def p4_A(tt):
                s = tt % 3
                ps = tt % 2
                rs = slice(tt * 128, (tt + 1) * 128)
                t_h = s_hl[s].dma(SP, lambda: nc.sync.dma_start(out=hld[s][:], in_=hbuf[rs, :]), hld_free[s])
                PE.wait(t_wo, pM_free[ps])
                for nb in range(4):
                    for ec in range(16):
                        ins = nc.tensor.matmul(pM[ps * 4 + nb][:, :], lhsT=catT[:, ec, rs], rhs=wo[:, ec, nb * 512:(nb + 1) * 512], start=(ec == 0), stop=(ec == 15))
                tM = PE.done(ins)
                DVE.wait(t_h, tM)
                for nb in range(4):
                    cs = slice(nb * 512, (nb + 1) * 512)
                    t = DVE.op(lambda: nc.vector.scalar_tensor_tensor(out=hld[s][:, cs], in0=hld[s][:, cs], scalar=ALPHA, in1=pM[ps * 4 + nb][:, :], op0=ALU.mult, op1=ALU.add))
                pM_free[ps] = t
                return t

            t_As = {0: p4_A(0)}
            for tt in range(16):
                s = tt % 3
                hs = tt % 2
                rs = slice(tt * 128, (tt + 1) * 128)
                t_n = ln_norm_act(hld[s], t_As[tt], lnp[s])
                if tt + 1 < 16:
                    t_As[tt + 1] = p4_A(tt + 1)
                t = DVE.op(lambda: nc.vector.tensor_tensor(out=hld[s][:], in0=hld[s][:], in1=l1[:, 0, :], op=ALU.mult), t_n, t_l1)
                t_f = POOL.op(lambda: nc.gpsimd.tensor_tensor(out=h1f[hs][:], in0=hld[s][:], in1=l1[:, 1, :], op=ALU.add), t, h1f_free[hs])
                hld_free[s] = t_f
                t_b16 = ACT.op(lambda: nc.scalar.copy(out=h1b[hs][:], in_=h1f[hs][:]), t_f, h1b_free[hs])
                t_s1 = s_o1[hs].dma(SP, lambda: nc.sync.dma_start(out=h1buf[rs, :], in_=h1f[hs][:]), t_f)
                t_s2 = s_o2[hs].dma(SP, lambda: nc.sync.dma_start(out=h1bf[rs, :], in_=h1b[hs][:]), t_b16)
                h1f_free[hs] = [t_b16, t_s1]
                h1b_free[hs] = t_s2
            barrier()
        ph_cat.close()
        if stage == 2:
            SP.wait([sl.tok() for sl in Slot.allslots])
            return nc

        G_all = sb("G_all", [128, 16, NE], F32)
        selm_bf = sb("selm_bf", [128, 16, NE], BF16)
        selm_f = sb("selm_f", [128, 16, NE], F32)
        slot8_all = sb("slot8_all", [128, 16, 8], I32)
        gate8_all = sb("gate8_all", [128, 16, 8], F32)
        with ExitStack() as ph_moe:
            wgb = [sb(f"wgb{i}", [128, 16, FH], BF16, ph_moe) for i in range(2)]
            wub = [sb(f"wub{i}", [128, 16, FH], BF16, ph_moe) for i in range(2)]
            wdb = [sb(f"wdb{i}", [128, 4, D], BF16, ph_moe) for i in range(2)]
            xg1 = sb("xg1", [128, 4, D], BF16, ph_moe)
            xg = [xg1, xg1]
            xT = [sb(f"xT{i}", [128, 16, CAP], BF16, ph_moe) for i in range(2)]
            hid = sb("hid", [128, 4, CAP], BF16, ph_moe)
            sg = [sb(f"sg{i}", [128, CAP], F32, ph_moe) for i in range(2)]
            yo = [sb(f"yo{i}", [128, D], BF16, ph_moe) for i in range(2)]
            pT2 = pst("pT2", [128, 1024], BF16, ph_moe)
            pGm = pst("pGm", [128, 512], F32, ph_moe)
            pUm = pst("pUm", [128, 512], F32, ph_moe)
            pYm = [pst(f"pYm{i}", [128, 512], F32, ph_moe) for i in range(4)]
            pR = pst("pR", [128, 512], F32, ph_moe)
            s_wgu = [Slot(nc, f"wgu{i}", st) for i in range(2)]
            s_wd = [Slot(nc, f"wd{i}", st) for i in range(2)]
            s_xg = [Slot(nc, f"xg{i}", st) for i in range(2)]
            s_yo = [Slot(nc, f"yo{i}", st) for i in range(2)]
            fst = dict(wgu_free=[None, None], wd_free=[None, None], xg_free=[None, None], xT_free=[None, None], pT2_free=[None, None], itp=0, tp_banks=[pT2[:, :]], pG_free=None, pU_free=None,
                       hid_free=None, sg_free=[None, None], pY_free=[None] * 4, yo_free=[None, None], isg=0, iyo=0)

            def load_w(s, wg_ap, wu_ap, wd_ap):
                t = s_wgu[s].dma(POOL, lambda: nc.gpsimd.dma_start(out=wgb[s][:].rearrange("p c f -> p (c f)"), in_=wg_ap.rearrange("(p c) f -> p (c f)", c=16), max_dma_last_dim=8192), fst["wgu_free"][s])
                t_gu = s_wgu[s].dma(POOL, lambda: nc.gpsimd.dma_start(out=wub[s][:].rearrange("p c f -> p (c f)"), in_=wu_ap.rearrange("(p c) f -> p (c f)", c=16), max_dma_last_dim=8192))
                t_d = s_wd[s].dma(POOL, lambda: nc.gpsimd.dma_start(out=wdb[s][:], in_=wd_ap.rearrange("(c p) n -> p c n", p=128)), fst["wd_free"][s])
                return t_gu, t_d

            def transpose_block(s, ntile, t_x, j0=0):
                for j in range(ntile):
                    for g in range(2):
                        banks = fst["tp_banks"]
                        bi = fst["itp"] % len(banks)
                        fst["itp"] += 1
                        pTb = banks[bi]
                        PE.wait(t_x, fst["pT2_free"][bi], fst["xT_free"][s])
                        for i in range(8):
                            dc = g * 8 + i
                            ins = nc.tensor.transpose(out=pTb[:, i * 128:(i + 1) * 128], in_=xg1[:, j0 + j, dc:D:16], identity=ident_bf[:])
                        tp = PE.done(ins)
                        src = pTb[:, :].rearrange("p (a b) -> p a b", a=8)
                        if g == 0:
                            t = ACT.op(lambda: nc.scalar.copy(out=xT[s][:, 0:8, j * 128:(j + 1) * 128], in_=src), tp, fst["xT_free"][s])
                        else:
                            t = DVE.op(lambda: nc.vector.tensor_copy(out=xT[s][:, 8:16, j * 128:(j + 1) * 128], in_=src), tp, fst["xT_free"][s])
                        fst["pT2_free"][bi] = t
                fst["xg_free"][s] = tp
                return [ACT.last, DVE.last]

            def ffn_gu(s, xs, W, t_w, t_xT):
                for fc in range(4):
                    PE.wait(t_w, t_xT, fst["pG_free"])
                    for dc in range(16):
                        ins = nc.tensor.matmul(pGm[:, 0:W], lhsT=wgb[s][:, dc, fc * 128:(fc + 1) * 128], rhs=xT[xs][:, dc, 0:W], start=(dc == 0), stop=(dc == 15))
                    tG = PE.done(ins)
                    PE.wait(fst["pU_free"])
                    for dc in range(16):
                        ins = nc.tensor.matmul(pUm[:, 0:W], lhsT=wub[s][:, dc, fc * 128:(fc + 1) * 128], rhs=xT[xs][:, dc, 0:W], start=(dc == 0), stop=(dc == 15))
                    tU = PE.done(ins)
                    q = fst["isg"] % 2
                    fst["isg"] += 1
                    t_sg = ACT.op(lambda: nc.scalar.activation(out=sg[q][:, 0:W], in_=pGm[:, 0:W], func=AF.Silu), tG, fst["sg_free"][q])
                    fst["pG_free"] = t_sg
                    t_h = DVE.op(lambda: nc.vector.tensor_tensor(out=hid[:, fc, 0:W], in0=pUm[:, 0:W], in1=sg[q][:, 0:W], op=ALU.mult), tU, t_sg, fst["hid_free"])
                    fst["pU_free"] = t_h
                    fst["sg_free"][q] = t_h
                fst["xT_free"][xs] = tU
                fst["wgu_free"][s] = tU
                return t_h

            def ffn_d(s, W, t_wd, t_h, store):
                last = None
                for j in range((W + 127) // 128):
                    q = fst["iyo"] % 2
                    fst["iyo"] += 1
                    nr = min(128, W - j * 128)
                    tys = []
                    for nb in range(4):
                        PE.wait(t_h, t_wd, fst["pY_free"][nb])
                        for fc in range(4):
                            ins = nc.tensor.matmul(pYm[nb][0:nr, :], lhsT=hid[:, fc, j * 128:j * 128 + nr], rhs=wdb[s][:, fc, nb * 512:(nb + 1) * 512], start=(fc == 0), stop=(fc == 3))
                        tY = PE.done(ins)
                        cs = slice(nb * 512, (nb + 1) * 512)
                        if nb % 2 == 0:
                            te = ACT.op(lambda: nc.scalar.copy(out=yo[q][0:nr, cs], in_=pYm[nb][0:nr, :]), tY, fst["yo_free"][q])
                        else:
                            te = DVE.op(lambda: nc.vector.tensor_copy(out=yo[q][0:nr, cs], in_=pYm[nb][0:nr, :]), tY, fst["yo_free"][q])
                        fst["pY_free"][nb] = te
                        tys.append(te)
                        last = tY
                    fst["yo_free"][q] = store(j, yo[q], tys, s_yo[q])
                fst["hid_free"] = last
                fst["wd_free"][s] = last

            tokidx_all = sb("tokidx_all", [128, NE * NTL, 4], I32, ph_moe)
            with ExitStack() as ph:
                wr_b = sb("wr_b", [128, 16, NE], BF16, ph)
                rb_row = sb("rb_row", [128, NE], F32, ph)
                sc2 = sb("sc2", [128, 2, NE], F32, ph)
                selv = sb("selv", [128, 2, NE], F32, ph)
                m8 = sb("m8", [128, 2, 8, 8], F32, ph)
                gs = sb("gs", [128, 2, 8], F32, ph)
                gs8 = sb("gs8", [128, 2, 8], F32, ph)
                gm = sb("gm", [128, 2, 8], F32, ph)
                gm1 = sb("gm1", [128, 2, 8], F32, ph)
                msk = sb("msk", [128, 2, NE], F32, ph)
                t8 = sb("t8", [128, 2, 8], F32, ph)
                wv = sb("wv", [128, 2, NE], F32, ph)
                den = sb("den", [128, 2, 2], F32, ph)
                zt = sb("zt", [128, 1028], I32, ph)
                tokrow = sb("tokrow", [128, 16, 4], I32, ph)
                av = sb("av", [128, NE], F32, ph)
                vals = sb("vals", [128, NE], F32, ph)
                s8 = sb("s8", [128, 8], F32, ph)
                s8b = sb("s8b", [128, 8], F32, ph)
                e8 = sb("e8", [128, 8], F32, ph)
                junk = sb("junk", [128, NE], F32, ph)
                dump1 = sb("dump1", [128, 1], F32, ph)
                s_r = Slot(nc, "rt", st)
                s_r2 = Slot(nc, "rt2", st)
                s_z = Slot(nc, "z", st)
                s_sc = Slot(nc, "scat", st)
                s_ti = [Slot(nc, f"tix{i}", st) for i in range(8)]
                t_r = s_r.dma(POOL, lambda: nc.gpsimd.dma_start(out=wr_b[:], in_=w_router.rearrange("(p c) e -> p c e", c=16)))
                t_r = [t_r, s_r2.dma(SP, lambda: nc.sync.dma_start(out=rb_row[:], in_=router_bias[0:1, :].partition_broadcast(128)))]
                t = POOL.op(lambda: nc.gpsimd.memset(zt[:], 0))
                t = POOL.op(lambda: nc.gpsimd.memset(tokrow[:], 0), t)
                stz = slot_tok.rearrange("(p r) c -> p (r c)", p=128)
                t_z = s_z.dma(SP, lambda: nc.sync.dma_start(out=stz[:, :], in_=zt[:]), t)
                t_tr = DVE.op(lambda: nc.vector.tensor_copy(out=tokrow[:, :, 0], in_=cst[:, 512:528]), t)
                t_d1 = DVE.op(lambda: nc.vector.tensor_scalar(out=dump1[:], in0=cst[:, 528:529], scalar1=1.0, scalar2=None, op0=ALU.add))
                t_wgu, t_wd = load_w(0, wsg, wsu, wsd)
                t_ws = {}
                if stage >= 5:
                    t_ws[0] = load_w(1, w_gate[0], w_up[0], w_down[0])

                def lvl(fns, *deps):
                    DVE.wait(*deps)
                    t = None
                    for fn in fns:
                        t = DVE.done(fn())
                    return t

                t_pR_free = None
                t_sc = None
                for tb in range(8):
                    s = tb % 2
                    g0 = tb * 2
                    t_x = s_xg[s].dma(SP, lambda: nc.sync.dma_start(out=xg1[:, 2 * s:2 * s + 2, :], in_=h1bf[tb * 256:(tb + 1) * 256, :].rearrange("(j p) d -> p j d", p=128)), fst["xg_free"][s])
                    t_xT = transpose_block(s, 2, t_x, j0=2 * s)
                    PE.wait(t_xT, t_r, t_pR_free)
                    for j in range(2):
                        for dc in range(16):
                            ins = nc.tensor.matmul(pR[:, j * NE:(j + 1) * NE], lhsT=xT[s][:, dc, j * 128:(j + 1) * 128], rhs=wr_b[:, dc, :], start=(dc == 0), stop=(dc == 15))
                    tR = PE.done(ins)
                    t_sig = ACT.op(lambda: nc.scalar.activation(out=sc2[:].rearrange("p a b -> p (a b)"), in_=pR[:, 0:2 * NE], func=AF.Sigmoid), tR, t_pR_free)
                    t = lvl([lambda j=j: nc.vector.tensor_tensor(out=selv[:, j, :], in0=sc2[:, j, :], in1=rb_row[:], op=ALU.add) for j in range(2)], t_sig, t_r)
                    t = lvl([lambda j=j, g=g: nc.vector.max(out=m8[:, j, g, :], in_=selv[:, j, g * 8:(g + 1) * 8]) for j in range(2) for g in range(8)], t)
                    t = lvl([lambda: nc.vector.tensor_tensor(out=gs[:], in0=m8[:, :, :, 0], in1=m8[:, :, :, 1], op=ALU.add)], t)
                    t = lvl([lambda j=j: nc.vector.max(out=gs8[:, j, :], in_=gs[:, j, :]) for j in range(2)], t)
                    t = lvl([lambda j=j: nc.vector.tensor_scalar(out=gm[:, j, :], in0=gs[:, j, :], scalar1=gs8[:, j, 3:4], scalar2=None, op0=ALU.is_ge) for j in range(2)], t)
                    t = lvl([lambda: nc.vector.tensor_scalar(out=gm1[:], in0=gm[:], scalar1=-1.0, scalar2=None, op0=ALU.add)], t)
                    t = lvl([lambda j=j, g=g: nc.vector.tensor_scalar(out=msk[:, j, g * 8:(g + 1) * 8], in0=selv[:, j, g * 8:(g + 1) * 8], scalar1=gm[:, j, g:g + 1], scalar2=gm1[:, j, g:g + 1], op0=ALU.mult, op1=ALU.add) for j in range(2) for g in range(8)], t)
                    t = lvl([lambda j=j: nc.vector.max(out=t8[:, j, :], in_=msk[:, j, :]) for j in range(2)], t)
                    t = lvl([lambda j=j: nc.vector.tensor_scalar(out=selm_f[:, g0 + j, :], in0=msk[:, j, :], scalar1=t8[:, j, 7:8], scalar2=None, op0=ALU.is_ge) for j in range(2)], t)
                    t_selm = lvl([lambda: nc.vector.tensor_copy(out=selm_bf[:, g0:g0 + 2, :], in_=selm_f[:, g0:g0 + 2, :]),
                                  lambda: nc.vector.tensor_tensor(out=wv[:], in0=sc2[:], in1=selm_f[:, g0:g0 + 2, :], op=ALU.mult)], t)
                    t = lvl([lambda: nc.vector.tensor_reduce(out=den[:, 0, :], in_=wv[:], axis=mybir.AxisListType.X, op=ALU.add)], t_selm)
                    t = lvl([lambda: nc.vector.reciprocal(out=den[:, 1, :], in_=den[:, 0, :])], t)
                    t_G = lvl([lambda j=j: nc.vector.tensor_scalar(out=G_all[:, g0 + j, :], in0=wv[:, j, :], scalar1=den[:, 1, j:j + 1], scalar2=2.5, op0=ALU.mult, op1=ALU.mult) for j in range(2)], t)

                    def store_sh(j, yot, tys, slot, tb=tb):
                        r0 = (tb * 2 + j) * 128
                        return slot.dma(SP, lambda: nc.sync.dma_start(out=yshbuf[r0:r0 + 128, :], in_=yot[:]), tys)
                    t_h = ffn_gu(0, s, 256, t_wgu, t_xT)
                    ffn_d(0, 256, t_wd, t_h, store_sh)
                    PE.wait(t_selm, t_sig)
                    for j in range(2):
                        tt = g0 + j
                        o = pR[:, 128 + j * NE:128 + (j + 1) * NE]
                        ins = nc.tensor.matmul(o, lhsT=lstr_bf[:], rhs=selm_bf[:, tt, :], start=True, stop=(tt == 0))
                        for i in range(tt):
                            ins = nc.tensor.matmul(o, lhsT=ones_bf[:], rhs=selm_bf[:, i, :], start=False, stop=(i == tt - 1))
                    tP = PE.done(ins)
                    t = t_G
                    for j in range(2):
                        tt = g0 + j
                        pos = pR[:, 128 + j * NE:128 + (j + 1) * NE]
                        t = DVE.op(lambda: nc.vector.scalar_tensor_tensor(out=av[:], in0=pos, scalar=float(CAPV), in1=selm_f[:, tt, :], op0=ALU.is_lt, op1=ALU.mult), tP, t)
                        t = DVE.op(lambda: nc.vector.tensor_tensor(out=vals[:], in0=pos, in1=cst[:, 529:593], op=ALU.add), t)
                        t_pR_free = t
                        t = DVE.op(lambda: nc.vector.tensor_tensor(out=vals[:], in0=vals[:], in1=av[:], op=ALU.mult), t)
                        t = DVE.op(lambda: nc.vector.tensor_scalar(out=vals[:], in0=vals[:], scalar1=-1.0, scalar2=None, op0=ALU.add), t)
                        t = DVE.op(lambda: nc.vector.max(out=s8[:], in_=vals[:]), t)
                        t = DVE.op(lambda: nc.vector.tensor_scalar(out=s8b[:], in0=s8[:], scalar1=0.0, scalar2=dump1[:, 0:1], op0=ALU.is_lt, op1=ALU.mult), t, t_d1)
                        t = DVE.op(lambda: nc.vector.tensor_tensor(out=s8b[:], in0=s8b[:], in1=s8[:], op=ALU.add), t)
                        t_s8 = DVE.op(lambda: nc.vector.tensor_copy(out=slot8_all[:, tt, :], in_=s8b[:]), t)
                        t = DVE.op(lambda: nc.vector.scalar_tensor_tensor(out=vals[:], in0=cst[:, 384:448], scalar=1.0, in1=av[:], op0=ALU.add, op1=ALU.mult), t_s8)
                        t = DVE.op(lambda: nc.vector.tensor_scalar(out=vals[:], in0=vals[:], scalar1=-1.0, scalar2=None, op0=ALU.add), t)
                        t = DVE.op(lambda: nc.vector.max(out=e8[:], in_=vals[:]), t)
                        for k in range(8):
                            t = DVE.op(lambda: nc.vector.scalar_tensor_tensor(out=junk[:], in0=cst[:, 384:448], scalar=e8[:, k:k + 1], in1=G_all[:, tt, :], op0=ALU.is_equal, op1=ALU.mult, accum_out=gate8_all[:, tt, k:k + 1]), t)
                        for k in range(8):
                            t_sc = s_sc.dma(POOL, lambda: nc.gpsimd.indirect_dma_start(out=slot_tok[:, :], out_offset=bass.IndirectOffsetOnAxis(ap=slot8_all[:, tt, k:k + 1], axis=0), in_=tokrow[:, tt, :], in_offset=None), t_s8, t_z, t_tr)
                if stage >= 5:
                    t_ws[1] = load_w(0, w_gate[1], w_up[1], w_down[1])
                stv = slot_tok[0:NS, :].rearrange("(r p) c -> p r c", p=128)
                t_tis = []
                for i in range(8):
                    t_tis.append(s_ti[i].dma(SP, lambda: nc.sync.dma_start(out=tokidx_all[:, i * 32:(i + 1) * 32, :], in_=stv[:, i * 32:(i + 1) * 32, :]), t_sc))
                barrier(exclude=s_wgu + s_wd + (s_ti if stage >= 5 else []))
            if stage in (3, 4):
                s_d = Slot(nc, "dbg", st)
                t = s_d.dma(SP, lambda: nc.sync.dma_start(out=dbg_gate[:, :], in_=G_all[:].rearrange("p a b -> p (a b)")))
                if stage == 4:
                    t = s_d.dma(SP, lambda: nc.sync.dma_start(out=dbg_s8[:, :], in_=slot8_all[:].rearrange("p a b -> p (a b)")))
                SP.wait(t, [sl.tok() for sl in Slot.allslots])
                return nc

            s_xgp = [Slot(nc, f"xgp{i}", st) for i in range(2)]
            fst["tp_banks"] = [pT2[:, :], pR[:, :].bitcast(BF16)]
            fst["itp"] = 0

            def gather_e(e):
                s = e % 2
                t_x = None
                for m in range(NTL):
                    t_x = s_xgp[s].dma(POOL, lambda: nc.gpsimd.indirect_dma_start(out=xg1[:, m, :], out_offset=None, in_=h1bf[:, :], in_offset=bass.IndirectOffsetOnAxis(ap=tokidx_all[:, e * NTL + m, 0:1], axis=0)), (fst["xg_free"][0], fst["xg_free"][1], t_tis[e // 8]) if m == 0 else None)
                return t_x

            t_xg = {0: gather_e(0)}
            t_xTs = {0: transpose_block(0, NTL, t_xg[0])}
            for e in range(NE):
                s = e % 2
                ws = (e + 1) % 2
                if e + 1 < NE:
                    t_xg[e + 1] = gather_e(e + 1)
                    if e + 1 >= 2:
                        t_ws[e + 1] = load_w((e + 2) % 2, w_gate[e + 1], w_up[e + 1], w_down[e + 1])
                t_h = ffn_gu(ws, s, CAPV, t_ws[e][0], t_xTs[e])
                if e + 1 < NE:
                    t_xTs[e + 1] = transpose_block((e + 1) % 2, NTL, t_xg[e + 1])

                def store_e(j, yot, tys, slot, e=e):
                    r0 = e * CAP + j * 128
                    return slot.dma(SP, lambda: nc.sync.dma_start(out=ybuf[r0:r0 + 128, :], in_=yot[:, :]), tys)
                ffn_d(ws, CAPV, t_ws[e][1], t_h, store_e)
            barrier()

        with ExitStack() as ph:
            l2 = sb("l2", [128, 2, D], F32, ph)
            acc = [sb(f"acc{i}", [128, D], F32, ph) for i in range(4)]
            ysh = [sb(f"ysh{i}", [128, D], BF16, ph) for i in range(3)]
            yg = [sb(f"yg{i}", [128, D], BF16, ph) for i in range(24)]
            zf = sb("zf", [128, D], BF16, ph)
            dgt = [sb(f"dgt{i}", [128, 16, 128], BF16, ph) for i in range(3)]
            ghl = [sb(f"ghl{i}", [128, 2, 8], F32, ph) for i in range(3)]
            ghb = [sb(f"ghb{i}", [128, 8], BF16, ph) for i in range(3)]
            pC = [pst(f"pC{i}", [128, 512], F32, ph) for i in range(8)]
            lnp = [(sb(f"st7{i}", [128, 24], F32, ph), sb(f"mv7{i}", [128, 2], F32, ph), sb(f"sd7{i}", [128, 4], F32, ph)) for i in range(4)]
            s_l2 = Slot(nc, "l2", st)
            s_zf = Slot(nc, "zf", st)
            s_a = [Slot(nc, f"a{i}", st) for i in range(4)]
            s_ys = [Slot(nc, f"ys{i}", st) for i in range(3)]
            s_yg = [Slot(nc, f"yg{i}", st) for i in range(24)]
            s_out = [Slot(nc, f"out{i}", st) for i in range(4)]
            t_l2 = s_l2.dma(SP, lambda: nc.sync.dma_start(out=l2[:, 0, :], in_=ln12[2:3, :].partition_broadcast(128)))
            t_l2 = s_l2.dma(SP, lambda: nc.sync.dma_start(out=l2[:, 1, :], in_=ln12[3:4, :].partition_broadcast(128)))
            t = POOL.op(lambda: nc.gpsimd.memset(zf[:], 0.0))
            t_zf = s_zf.dma(SP, lambda: nc.sync.dma_start(out=ybuf[NS:NS + 128, :], in_=zf[:]), t)
            acc_free = [None] * 4
            ysh_free = [None] * 3
            yg_free = [None] * 24
            dgt_free = [None] * 3
            pC_free = [None, None]
            tk = {}

            def p7_A0(tt):
                s = tt % 4
                ys = tt % 3
                rs = slice(tt * 128, (tt + 1) * 128)
                t_a = s_a[s].dma(SP, lambda: nc.sync.dma_start(out=acc[s][:], in_=h1buf[rs, :]), acc_free[s])
                t_y = s_ys[ys].dma(SP, lambda: nc.sync.dma_start(out=ysh[ys][:], in_=yshbuf[rs, :]), ysh_free[ys])
                t_gs = []
                for k in range(8):
                    q = ys * 8 + k
                    t_gs.append(s_yg[q].dma(POOL, lambda: nc.gpsimd.indirect_dma_start(out=yg[q][:], out_offset=None, in_=ybuf[:, :], in_offset=bass.IndirectOffsetOnAxis(ap=slot8_all[:, tt, k:k + 1], axis=0)), yg_free[q], t_zf))
                t = DVE.op(lambda: nc.vector.tensor_copy(out=ghb[ys][:], in_=gate8_all[:, tt, :]), dgt_free[ys])
                t = DVE.op(lambda: nc.vector.tensor_copy(out=ghl[ys][:, 0, :], in_=ghb[ys][:]), t)
                t = DVE.op(lambda: nc.vector.tensor_tensor(out=ghl[ys][:, 1, :], in0=gate8_all[:, tt, :], in1=ghl[ys][:, 0, :], op=ALU.subtract), t)
                ACT.wait(t, dgt_free[ys])
                for i in range(16):
                    t_dg = ACT.done(nc.scalar.activation(out=dgt[ys][:, i, :], in_=ident_bf[:], func=AF.Copy, scale=ghl[ys][:, i // 8, i % 8:i % 8 + 1]))
                t_base = DVE.op(lambda: nc.vector.scalar_tensor_tensor(out=acc[s][:], in0=acc[s][:], scalar=ALPHA, in1=ysh[ys][:], op0=ALU.mult, op1=ALU.add), t_a, t_y)
                ysh_free[ys] = t_base
                tk[tt] = (t_gs, t_dg, t_base)

            def p7_A1(tt):
                s = tt % 4
                ys = tt % 3
                ps = tt % 2
                t_gs, t_dg, t_base = tk[tt]
                PE.wait(t_dg, pC_free[ps])
                for k in range(8):
                    PE.wait(t_gs[k])
                    for hl in range(2):
                        for nb in range(4):
                            ins = nc.tensor.matmul(pC[ps * 4 + nb][:, :], lhsT=dgt[ys][:, hl * 8 + k, :], rhs=yg[ys * 8 + k][:, nb * 512:(nb + 1) * 512], start=(k == 0 and hl == 0), stop=(k == 7 and hl == 1))
                tC = PE.done(ins)
                for k in range(8):
                    yg_free[ys * 8 + k] = tC
                dgt_free[ys] = tC
                DVE.wait(tC, t_base)
                for nb in range(4):
                    cs = slice(nb * 512, (nb + 1) * 512)
                    t = DVE.done(nc.vector.tensor_tensor(out=acc[s][:, cs], in0=acc[s][:, cs], in1=pC[ps * 4 + nb][:, :], op=ALU.add))
                pC_free[ps] = t
                return t

            p7_A0(0)
            p7_A0(1)
            t_As = {0: p7_A1(0)}
            for tt in range(16):
                s = tt % 4
                rs = slice(tt * 128, (tt + 1) * 128)
                if tt + 2 < 16:
                    p7_A0(tt + 2)
                t_n = ln_norm_act(acc[s], t_As[tt], lnp[s])
                t = DVE.op(lambda: nc.vector.tensor_tensor(out=acc[s][:], in0=acc[s][:], in1=l2[:, 0, :], op=ALU.mult), t_n, t_l2)
                t = DVE.op(lambda: nc.vector.tensor_tensor(out=acc[s][:], in0=acc[s][:], in1=l2[:, 1, :], op=ALU.add), t)
                acc_free[s] = s_out[s].dma(ACT, lambda: nc.scalar.dma_start(out=y[rs, :], in_=acc[s][:]), t)
                if tt + 1 < 16:
                    t_As[tt + 1] = p7_A1(tt + 1)
            SP.wait([sl.tok() for sl in Slot.allslots])
    return nc


def prep_inputs(inp, stage=99):
    f = np.float32
    x = np.asarray(inp["x"], f)
    common = {}
    cst = np.zeros((128, NCONST), f)
    cst[:, 0:128] = np.eye(128, dtype=f)
    cst[:, 128:256] = np.triu(np.ones((128, 128), f), 1)
    cst[:, 256:384] = 1.0
    cst[:, 384:448] = np.arange(64, dtype=f)[None, :]
    cst[:, 448:512] = (np.arange(64, dtype=f) * CAP)[None, :]
    cst[:, 512:528] = np.arange(16, dtype=f)[None, :] * 128 + np.arange(128, dtype=f)[:, None]
    cst[:, 528] = NS + np.arange(128, dtype=f)
    cst[:, 529:529 + 64] = np.arange(64, dtype=f)[None, :] * CAP + 1.0
    common["consts"] = cst
    common["ln_in_gb"] = np.stack([np.asarray(inp["ln_in_g"], f), np.asarray(inp["ln_in_b"], f)])
    common["w_in"] = np.ascontiguousarray(np.asarray(inp["w_in"], f)[0])
    cw = np.asarray(inp["conv_w"], f)[0]
    common["conv_wT"] = np.ascontiguousarray(cw.T.reshape(8, 128, 31).transpose(1, 0, 2).reshape(128, 8 * 31))
    cols = lambda v: np.asarray(v, f)[0].reshape(8, 128).T
    common["conv_par"] = np.ascontiguousarray(np.concatenate([cols(inp["conv_b"]), cols(inp["conv_ln_g"]), cols(inp["conv_ln_b"])], axis=1))
    rb = np.asarray(inp["rel_bias"], f)[0]
    qi = np.arange(128)
    kk = np.arange(640)
    rel = 512 + qi[None, :] - kk[:, None]
    idx = np.clip(rel, -63, 256) + 63
    bias = rb[:, idx]
    kloc = kk[:, None] - 64 * (qi[None, :] >= 64)
    valid = (kloc >= 0) & (kloc < 576)
    bias = np.where(valid[None], bias, f(-30000.0)).astype(f)
    common["biasT"] = np.ascontiguousarray(bias.reshape(16, 5, 128, 128).transpose(2, 0, 1, 3).reshape(128, 16 * 5 * 128))
    common["w_out"] = np.ascontiguousarray(np.asarray(inp["w_out"], f)[0])
    common["ln12"] = np.stack([np.asarray(inp[k], f)[0] for k in ("ln1_g", "ln1_b", "ln2_g", "ln2_b")])
    common["w_router"] = np.ascontiguousarray(np.asarray(inp["w_router"], f)[0])
    common["router_bias"] = np.asarray(inp["router_bias"], f).reshape(1, NE)
    if stage >= 5:
        common["w_gate"] = np.asarray(inp["w_gate"], f)[0]
        common["w_up"] = np.asarray(inp["w_up"], f)[0]
        common["w_down"] = np.asarray(inp["w_down"], f)[0]
    common["wsg"] = np.asarray(inp["w_shared_gate"], f)[0]
    common["wsu"] = np.asarray(inp["w_shared_up"], f)[0]
    common["wsd"] = np.asarray(inp["w_shared_down"], f)[0]
    maps = []
    for c in range(8):
        b, half = c // 2, c % 2
        xs = np.zeros((NTH, D), f)
        if half == 1:
            xs[:] = x[b, NT - HALO:2 * NT]
        else:
            xs[HALO:] = x[b, 0:NT]
        m = dict(common)
        m["x"] = xs
        m["flags"] = np.full((128, 2), float(half), f)
        maps.append(m)
    return maps


_NC_CACHE = {}


def kernel(**inputs):
    maps = prep_inputs(inputs)
    if "nc" not in _NC_CACHE:
        _NC_CACHE["nc"] = build_nc()
    res = run_bass_kernel_spmd(_NC_CACHE["nc"], maps, core_ids=list(range(8)))
    out = np.zeros((4, 2 * NT, D), np.float32)
    for c in range(8):
        b, half = c // 2, c % 2
        out[b, half * NT:(half + 1) * NT] = res.results[c]["y"]
    return out
```

```python
import numpy as np
from contextlib import ExitStack
import concourse.bass as bass
import concourse.mybir as mybir
from concourse.bass_utils import run_bass_kernel_spmd

F32, BF16, I32 = mybir.dt.float32, mybir.dt.bfloat16, mybir.dt.int32
AF = mybir.ActivationFunctionType
ALU = mybir.AluOpType

D = 2048
NT = 2048
HALO = 512
NTH = NT + HALO
NE = 64
CAP = 512
CAPV = 448
NTL = CAP // 128
NS = NE * CAP
FH = 512
ALPHA = 2.0 ** 0.25
EPS = 1e-5
NCONST = 640


class Tok:
    __slots__ = ("sem", "val")

    def __init__(self, sem, val):
        self.sem = sem
        self.val = val


def _flat(ts):
    for t in ts:
        if t is None:
            continue
        if isinstance(t, (list, tuple)):
            yield from _flat(t)
        else:
            yield t


class Eng:
    def __init__(self, nc, h, name, st):
        self.h = h
        self.sem = st.enter_context(nc.semaphore("s_" + name))
        self.n = 0
        self.seen = {}
        self.last = None

    def wait(self, *toks):
        for t in _flat(toks):
            k = id(t.sem)
            if self.seen.get(k, 0) < t.val:
                self.h.wait_ge(t.sem, t.val)
                self.seen[k] = t.val

    def done(self, ins):
        self.n += 1
        ins.then_inc(self.sem, 1)
        self.last = Tok(self.sem, self.n)
        return self.last

    def op(self, fn, *deps):
        self.wait(*deps)
        return self.done(fn())

    def raw(self, fn, *deps):
        self.wait(*deps)
        fn()


class Slot:
    allslots = []

    def __init__(self, nc, name, st):
        self.sem = st.enter_context(nc.semaphore("d_" + name))
        self.cnt = 0
        Slot.allslots.append(self)

    def dma(self, q, fn, *deps):
        q.wait(*deps)
        self.cnt += 16
        fn().then_inc(self.sem, 16)
        return Tok(self.sem, self.cnt)

    def tok(self):
        return Tok(self.sem, self.cnt) if self.cnt else None


def build_nc(stage=99):
    Slot.allslots = []
    nc = bass.Bass("TRN2", target_bir_lowering=False)

    def din(name, shape, dt=F32):
        return nc.dram_tensor(name, shape, dt, kind="ExternalInput").ap()

    x = din("x", [NTH, D])
    flags = din("flags", [128, 2])
    consts = din("consts", [128, NCONST])
    ln_in_gb = din("ln_in_gb", [2, D])
    w_in = din("w_in", [D, 5120])
    conv_wT = din("conv_wT", [128, 8 * 31])
    conv_par = din("conv_par", [128, 24])
    biasT = din("biasT", [128, 16 * 5 * 128])
    w_out = din("w_out", [D, D])
    ln12 = din("ln12", [4, D])
    w_router = din("w_router", [D, NE])
    router_bias = din("router_bias", [1, NE])
    if stage >= 5:
        w_gate = din("w_gate", [NE, D, FH])
        w_up = din("w_up", [NE, D, FH])
        w_down = din("w_down", [NE, FH, D])
    wsg = din("wsg", [D, FH])
    wsu = din("wsu", [D, FH])
    wsd = din("wsd", [FH, D])
    y = nc.dram_tensor("y", [NT, D], F32, kind="ExternalOutput").ap()

    def dscr(name, shape, dt, dbg=False):
        return nc.dram_tensor(name, shape, dt, kind="ExternalOutput" if dbg else "Internal").ap()

    hbuf = dscr("hbuf", [NT, D], F32)
    h1buf = dscr("h1buf", [NT, D], F32, dbg=(stage == 2))
    h1bf = dscr("h1bf", [NT, D], BF16)
    yshbuf = dscr("yshbuf", [NT, D], BF16, dbg=(stage == 3))
    slot_tok = dscr("slot_tok", [NS + 128, 4], I32, dbg=(stage == 4))
    ybuf = dscr("ybuf", [NS + 128, D], BF16)
    gdram = dscr("gdram", [NT, NE], F32)
    dbg_cat = dscr("dbg_cat", [128, 16 * NT], BF16, dbg=(stage == 1)) if stage == 1 else None
    dbg_gate = dscr("dbg_gate", [128, 16 * 64], F32, dbg=True) if stage in (3, 4) else None
    dbg_s8 = dscr("dbg_s8", [128, 16 * 8], I32, dbg=True) if stage == 4 else None

    with ExitStack() as st:
        PE = Eng(nc, nc.tensor, "pe", st)
        ACT = Eng(nc, nc.scalar, "act", st)
        DVE = Eng(nc, nc.vector, "dve", st)
        POOL = Eng(nc, nc.gpsimd, "pool", st)
        SP = Eng(nc, nc.sync, "sp", st)
        engines = [PE, ACT, DVE, POOL, SP]

        def sb(name, shape, dt, stack=st):
            return stack.enter_context(nc.sbuf_tensor(name, shape, dt))

        def pst(name, shape, dt, stack):
            return stack.enter_context(nc.psum_tensor(name, shape, dt))

        def barrier(exclude=()):
            toks = [e.last for e in engines] + [s.tok() for s in Slot.allslots if s not in exclude]
            for e in engines:
                e.wait(toks)

        cst = sb("cst", [128, NCONST], F32)
        ident_bf = sb("ident_bf", [128, 128], BF16)
        lstr_bf = sb("lstr_bf", [128, 128], BF16)
        ones_bf = sb("ones_bf", [128, 128], BF16)
        flg = sb("flg", [128, 2], F32)
        eps_t = sb("eps_t", [128, 1], F32)
        ph_cat = ExitStack()
        catT = sb("catT", [128, 16, NT], BF16, ph_cat)
        s_c = Slot(nc, "c", st)
        t_c = s_c.dma(SP, lambda: nc.sync.dma_start(out=cst[:], in_=consts[:, :]))
        t_c = s_c.dma(SP, lambda: nc.sync.dma_start(out=flg[:], in_=flags[:, :]))
        t0 = DVE.op(lambda: nc.vector.tensor_copy(out=ident_bf[:], in_=cst[:, 0:128]), t_c)
        t0 = DVE.op(lambda: nc.vector.tensor_copy(out=lstr_bf[:], in_=cst[:, 128:256]))
        t0 = DVE.op(lambda: nc.vector.tensor_copy(out=ones_bf[:], in_=cst[:, 256:384]))
        t_const = DVE.op(lambda: nc.vector.memset(eps_t[:], EPS))
        ones_f = cst[:, 256:384]
        hv = flg[:, 0:1]

        def ln_stats(xt, n, tok_in, pool):
            stats, mv, sd = pool
            DVE.wait(tok_in)
            nchunk = n // 512
            for j in range(nchunk):
                t = DVE.op(lambda j=j: nc.vector.bn_stats(out=stats[:, j * 6:(j + 1) * 6], in_=xt[:, j * 512:(j + 1) * 512]))
            t = DVE.op(lambda: nc.vector.bn_aggr(out=mv[:, 0:2], in_=stats[:, 0:6 * nchunk]), t)
            t = ACT.op(lambda: nc.scalar.activation(out=sd[:, 0:1], in_=mv[:, 1:2], func=AF.Sqrt, bias=eps_t[:, 0:1], scale=1.0), t, t_const)
            t = DVE.op(lambda: nc.vector.reciprocal(out=sd[:, 1:2], in_=sd[:, 0:1]), t)
            return mv[:, 0:1], sd[:, 1:2], t

        def ln_norm_act(buf, tok_in, pool):
            mean, rstd, t = ln_stats(buf, D, tok_in, pool)
            sd = pool[2]
            t = DVE.op(lambda: nc.vector.tensor_scalar(out=sd[:, 2:3], in0=mean, scalar1=rstd, scalar2=-1.0, op0=ALU.mult, op1=ALU.mult), t)
            return ACT.op(lambda: nc.scalar.activation(out=buf[:], in_=buf[:], func=AF.Identity, bias=sd[:, 2:3], scale=rstd), t)

        with ExitStack() as ph_mix:
            hT = sb("hT", [128, 16, NTH], BF16, ph_mix)
            with ExitStack() as ph:
                gbrow = sb("gbrow", [128, 2, D], F32, ph)
                xt = [sb(f"xt{i}", [128, D], F32, ph) for i in range(2)]
                hf = [sb(f"hf{i}", [128, D], F32, ph) for i in range(2)]
                hb = [sb(f"hb{i}", [128, D], BF16, ph) for i in range(2)]
                lnp = [(sb(f"st{i}", [128, 24], F32, ph), sb(f"mv{i}", [128, 2], F32, ph), sb(f"sd{i}", [128, 2], F32, ph)) for i in range(2)]
                ptr = [pst(f"ptr{i}", [128, 1024], BF16, ph) for i in range(4)]
                s_x = [Slot(nc, f"x{i}", st) for i in range(2)]
                s_h = [Slot(nc, f"h{i}", st) for i in range(2)]
                s_g = Slot(nc, "gb", st)
                t_g = s_g.dma(SP, lambda: nc.sync.dma_start(out=gbrow[:, 0, :], in_=ln_in_gb[0:1, :].partition_broadcast(128)))
                t_g = s_g.dma(SP, lambda: nc.sync.dma_start(out=gbrow[:, 1, :], in_=ln_in_gb[1:2, :].partition_broadcast(128)))
                x_free = [None, None]
                hf_free = [None, None]
                hb_free = [None, None]
                ptr_free = [None] * 4
                NTI = NTH // 128
                t_hfs = [None] * NTI
                for it in range(NTI + 1):
                    if it < NTI:
                        tt = it
                        s = tt % 2
                        t_ld = s_x[s].dma(SP, lambda: nc.sync.dma_start(out=xt[s][:], in_=x[tt * 128:(tt + 1) * 128, :]), x_free[s])
                        mean, rstd, t = ln_stats(xt[s], D, t_ld, lnp[s])
                        t = DVE.op(lambda: nc.vector.tensor_scalar(out=hf[s][:], in0=xt[s][:], scalar1=mean, scalar2=rstd, op0=ALU.subtract, op1=ALU.mult), t, hf_free[s])
                        x_free[s] = t
                        t = DVE.op(lambda: nc.vector.tensor_tensor(out=hf[s][:], in0=hf[s][:], in1=gbrow[:, 0, :], op=ALU.mult), t, t_g)
                        t_hfs[tt] = POOL.op(lambda: nc.gpsimd.tensor_tensor(out=hf[s][:], in0=hf[s][:], in1=gbrow[:, 1, :], op=ALU.add), t)
                    if it >= 1:
                        tt = it - 1
                        s = tt % 2
                        t_hf = t_hfs[tt]
                        t_hb = ACT.op(lambda: nc.scalar.copy(out=hb[s][:], in_=hf[s][:]), t_hf, hb_free[s])
                        if tt >= HALO // 128:
                            r0 = (tt - HALO // 128) * 128
                            t_st = s_h[s].dma(SP, lambda: nc.sync.dma_start(out=hbuf[r0:r0 + 128, :], in_=hf[s][:]), t_hf)
                            hf_free[s] = [t_hb, t_st]
                        else:
                            hf_free[s] = t_hb
                        tps = []
                        for g in range(2):
                            pi = (tt % 2) * 2 + g
                            PE.wait(t_hb, ptr_free[pi])
                            for j in range(8):
                                dc = g * 8 + j
                                ins = nc.tensor.transpose(out=ptr[pi][:, j * 128:(j + 1) * 128], in_=hb[s][:, dc * 128:(dc + 1) * 128], identity=ident_bf[:])
                            tp = PE.done(ins)
                            if g == 0:
                                te = ACT.op(lambda: nc.scalar.copy(out=hT[:, 0:8, tt * 128:(tt + 1) * 128], in_=ptr[pi][:, :].rearrange("p (a b) -> p a b", a=8)), tp)
                            else:
                                te = DVE.op(lambda: nc.vector.tensor_copy(out=hT[:, 8:16, tt * 128:(tt + 1) * 128], in_=ptr[pi][:, :].rearrange("p (a b) -> p a b", a=8)), tp)
                            ptr_free[pi] = te
                            tps.append(tp)
                        hb_free[s] = tps
                barrier()

            with ExitStack() as ph:
                wc = [sb(f"wc{i}", [128, 16, 256], BF16, ph) for i in range(2)]
                dg = [sb(f"dg{i}", [128, 31, 128], BF16, ph) for i in range(2)]
                vT = [sb(f"vT{i}", [128, 32 + NT], BF16, ph) for i in range(2)]
                cw = sb("cw", [128, 8 * 31], F32, ph)
                cpar = sb("cpar", [128, 24], F32, ph)
                sig = [sb(f"sig{i}", [128, 512], F32, ph) for i in range(2)]
                pA = [pst(f"pA{i}", [128, 512], F32, ph) for i in range(2)]
                pG = [pst(f"pG{i}", [128, 512], F32, ph) for i in range(2)]
                pY = [pst(f"pY{i}", [128, 512], F32, ph) for i in range(2)]
                pS = [pst(f"pS{i}", [128, 512], F32, ph) for i in range(2)]
                s_w = [Slot(nc, f"wc{i}", st) for i in range(2)]
                s_p = Slot(nc, "cpar", st)
                t_p = s_p.dma(SP, lambda: nc.sync.dma_start(out=cw[:], in_=conv_wT[:, :]))
                t_p = s_p.dma(SP, lambda: nc.sync.dma_start(out=cpar[:], in_=conv_par[:, :]))
                w_in_v = w_in.rearrange("(c p) n -> p c n", p=128)
                wc_free = [None, None]
                dg_free = [None, None]
                vT_free = [None, None]
                pA_free = [None, None]
                pG_free = [None, None]
                pY_free = [None, None]
                sig_free = [None, None]
                ia = 0
                iy = 0
                for ct in range(8):
                    s = ct % 2
                    t_w = s_w[s].dma(POOL, lambda: nc.gpsimd.dma_start(out=wc[s][:, :, 0:128], in_=w_in_v[:, :, ct * 128:(ct + 1) * 128]), wc_free[s])
                    t_w = s_w[s].dma(POOL, lambda: nc.gpsimd.dma_start(out=wc[s][:, :, 128:256], in_=w_in_v[:, :, 1024 + ct * 128:1024 + (ct + 1) * 128]))
                    POOL.wait(t_p, dg_free[s], t_const)
                    for k in range(31):
                        t_dg = POOL.op(lambda k=k: nc.gpsimd.tensor_scalar(out=dg[s][:, k, :], in0=ident_bf[:], scalar1=cw[:, ct * 31 + k:ct * 31 + k + 1], scalar2=1.0, op0=ALU.mult, op1=ALU.mult))
                    t_v_prev = None
                    last_pe = None
                    for tb in range(-1, 4):
                        if tb < 0:
                            n, c0 = 32, HALO - 32
                        else:
                            n, c0 = 512, HALO + tb * 512
                        a = ia % 2
                        ia += 1
                        PE.wait(t_w, pA_free[a])
                        for dc in range(16):
                            ins = nc.tensor.matmul(pA[a][:, 0:n], lhsT=wc[s][:, dc, 0:128], rhs=hT[:, dc, c0:c0 + n], start=(dc == 0), stop=(dc == 15))
                        tA = PE.done(ins)
                        PE.wait(pG_free[a])
                        for dc in range(16):
                            ins = nc.tensor.matmul(pG[a][:, 0:n], lhsT=wc[s][:, dc, 128:256], rhs=hT[:, dc, c0:c0 + n], start=(dc == 0), stop=(dc == 15))
                        tG = PE.done(ins)
                        t_sig = ACT.op(lambda: nc.scalar.activation(out=sig[a][:, 0:n], in_=pG[a][:, 0:n], func=AF.Sigmoid), tG, sig_free[a])
                        pG_free[a] = t_sig
                        if tb < 0:
                            t_v = DVE.op(lambda: nc.vector.scalar_tensor_tensor(out=vT[s][:, 0:32], in0=pA[a][:, 0:32], scalar=hv, in1=sig[a][:, 0:32], op0=ALU.mult, op1=ALU.mult), tA, t_sig, vT_free[s])
                        else:
                            t_v = DVE.op(lambda: nc.vector.tensor_tensor(out=vT[s][:, 32 + tb * 512:32 + (tb + 1) * 512], in0=pA[a][:, :], in1=sig[a][:, :], op=ALU.mult), tA, t_sig, vT_free[s])
                        pA_free[a] = t_v
                        sig_free[a] = t_v
                        if tb >= 0:
                            yb = iy % 2
                            iy += 1
                            PE.wait(t_v, t_v_prev, t_dg, pY_free[yb])
                            for k in range(31):
                                ins = nc.tensor.matmul(pY[yb][:, :], lhsT=dg[s][:, k, :], rhs=vT[s][:, 2 + tb * 512 + k:2 + tb * 512 + k + 512], start=(k == 0), stop=(k == 30))
                            tY = PE.done(ins)
                            last_pe = tY
                            t_e = ACT.op(lambda: nc.scalar.activation(out=catT[:, ct, tb * 512:(tb + 1) * 512], in_=pY[yb][:, :], func=AF.Identity, bias=cpar[:, ct:ct + 1], scale=1.0), tY, t_p)
                            pY_free[yb] = t_e
                        t_v_prev = t_v
                    wc_free[s] = last_pe
                    dg_free[s] = last_pe
                    vT_free[s] = last_pe
                barrier()
                sq = [sb(f"sq{i}", [128, 512], F32, ph) for i in range(2)]
                mean_s = sb("mean_s", [128, 512], F32, ph)
                rstd_s = sb("rstd_s", [128, 512], F32, ph)
                tmpv = sb("tmpv", [128, 512], F32, ph)
                zt = [sb(f"zt{i}", [128, 512], F32, ph) for i in range(2)]
                sq_free = [None, None]
                zt_free = [None, None]
                t_prev_norm = None
                for tb in range(4):
                    cs = slice(tb * 512, (tb + 1) * 512)
                    PE.wait(t_prev_norm)
                    for ct in range(8):
                        ins = nc.tensor.matmul(pS[0][:, :], lhsT=ones_bf[:], rhs=catT[:, ct, cs], start=(ct == 0), stop=(ct == 7))
                    tS1 = PE.done(ins)
                    for ct in range(8):
                        q = ct % 2
                        t_sq = DVE.op(lambda: nc.vector.tensor_tensor(out=sq[q][:], in0=catT[:, ct, cs], in1=catT[:, ct, cs], op=ALU.mult), sq_free[q])
                        PE.wait(t_sq)
                        ins = nc.tensor.matmul(pS[1][:, :], lhsT=ones_f, rhs=sq[q][:], start=(ct == 0), stop=(ct == 7))
                        sq_free[q] = PE.done(ins)
                    tS2 = sq_free[1]
                    t = DVE.op(lambda: nc.vector.tensor_scalar(out=mean_s[:], in0=pS[0][:, :], scalar1=1.0 / 1024, scalar2=None, op0=ALU.mult), tS1, t_prev_norm)
                    t = DVE.op(lambda: nc.vector.tensor_tensor(out=tmpv[:], in0=mean_s[:], in1=mean_s[:], op=ALU.mult), t)
                    t = DVE.op(lambda: nc.vector.scalar_tensor_tensor(out=tmpv[:], in0=pS[1][:, :], scalar=1.0 / 1024, in1=tmpv[:], op0=ALU.mult, op1=ALU.subtract), t, tS2)
                    t = ACT.op(lambda: nc.scalar.activation(out=tmpv[:], in_=tmpv[:], func=AF.Sqrt, bias=eps_t[:, 0:1], scale=1.0), t)
                    t_r = DVE.op(lambda: nc.vector.reciprocal(out=rstd_s[:], in_=tmpv[:]), t)
                    for ct in range(8):
                        q = ct % 2
                        t = DVE.op(lambda: nc.vector.tensor_tensor(out=zt[q][:], in0=catT[:, ct, cs], in1=mean_s[:], op=ALU.subtract), t_r, zt_free[q])
                        t = DVE.op(lambda: nc.vector.tensor_tensor(out=zt[q][:], in0=zt[q][:], in1=rstd_s[:], op=ALU.mult), t)
                        t = ACT.op(lambda: nc.scalar.activation(out=catT[:, ct, cs], in_=zt[q][:], func=AF.Silu, bias=cpar[:, 16 + ct:17 + ct], scale=cpar[:, 8 + ct:9 + ct]), t)
                        zt_free[q] = t
                    t_prev_norm = t
                barrier()

            with ExitStack() as ph:
                bT = [sb(f"bT{i}", [128, 1280], BF16, ph) for i in range(2)]
                wq = [sb(f"wq{i}", [128, 16, 384], BF16, ph) for i in range(2)]
                kT = [sb(f"kT{i}", [128, NTH], BF16, ph) for i in range(1)] * 2
                qTm = [sb(f"qTm{i}", [128, 2, NT], BF16, ph) for i in range(1)] * 2
                va = [sb(f"va{i}", [128, 20, 2, 65], BF16, ph) for i in range(1)] * 2
                pT = [sb(f"pT{i}", [128, 5, 128], BF16, ph) for i in range(2)]
                att = [sb(f"att{i}", [128, 128], BF16, ph) for i in range(2)]
                rinv = [sb(f"rinv{i}", [128, 1], F32, ph) for i in range(2)]

                pSc = [pst(f"pSc{i}", [128, 1024], F32, ph) for i in range(2)]
                pO = [pst(f"pO{i}", [128, 512], F32, ph) for i in range(2)]
                pP = [pst(f"pP{i}", [128, 512], F32, ph) for i in range(2)]
                pTr = pP[1][:, :].bitcast(BF16)
                s_b = [Slot(nc, f"bT{i}", st) for i in range(2)]
                s_wq = [Slot(nc, f"wq{i}", st) for i in range(2)]
                t_init = None
                for i in range(1):
                    t_init = POOL.op(lambda: nc.gpsimd.memset(qTm[i][64:128, 0, :], 0.0))
                    t_init = POOL.op(lambda: nc.gpsimd.memset(qTm[i][0:64, 1, :], 0.0))
                    t_init = POOL.op(lambda: nc.gpsimd.memset(va[i][:, 4:20, :, 64:65], 1.0))
                    t_init = POOL.op(lambda: nc.gpsimd.tensor_scalar(out=va[i][:, 0:4, :, 64:65], in0=va[i][:, 4:8, :, 64:65], scalar1=hv, scalar2=1.0, op0=ALU.mult, op1=ALU.mult), t_init)
                wq_free = [None, None]
                pP_free = [None, None]
                pSc_free = [None, None]
                pT_free = [None, None]
                pO_free = [None, None]
                att_free = [None, None]
                pTr_free = None
                ip = 0
                isc = 0
                iatt = 0
                prev_att = None
                for hp in range(8):
                    s = hp % 2
                    t_b = s_b[s].dma(POOL, lambda: nc.gpsimd.dma_start(out=bT[s][:, :], in_=biasT[:, hp * 1280:(hp + 1) * 1280]), wq_free[s])
                    for j in range(3):
                        c0 = 2048 + j * 1024 + hp * 128
                        t_w = s_wq[s].dma(POOL, lambda: nc.gpsimd.dma_start(out=wq[s][:, :, j * 128:(j + 1) * 128], in_=w_in_v[:, :, c0:c0 + 128]), wq_free[s] if j == 0 else None)
                    for blk in range(5):
                        p = ip % 2
                        ip += 1
                        PE.wait(t_w, pP_free[p])
                        for dc in range(16):
                            ins = nc.tensor.matmul(pP[p][:, :], lhsT=wq[s][:, dc, 128:256], rhs=hT[:, dc, blk * 512:(blk + 1) * 512], start=(dc == 0), stop=(dc == 15))
                        tP = PE.done(ins)
                        pP_free[p] = ACT.op(lambda: nc.scalar.copy(out=kT[s][:, blk * 512:(blk + 1) * 512], in_=pP[p][:, :]), tP, prev_att)
                    t_k = ACT.last
                    for blk in range(4):
                        p = ip % 2
                        ip += 1
                        PE.wait(pP_free[p])
                        for dc in range(16):
                            ins = nc.tensor.matmul(pP[p][:, :], lhsT=wq[s][:, dc, 0:128], rhs=hT[:, dc, HALO + blk * 512:HALO + (blk + 1) * 512], start=(dc == 0), stop=(dc == 15))
                        tP = PE.done(ins)
                        t1 = DVE.op(lambda: nc.vector.tensor_scalar(out=qTm[s][0:64, 0, blk * 512:(blk + 1) * 512], in0=pP[p][0:64, :], scalar1=0.125, scalar2=None, op0=ALU.mult), tP, prev_att, t_init)
                        t2 = DVE.op(lambda: nc.vector.tensor_scalar(out=qTm[s][64:128, 1, blk * 512:(blk + 1) * 512], in0=pP[p][64:128, :], scalar1=0.125, scalar2=None, op0=ALU.mult))
                        pP_free[p] = t2
                    t_q = t2
                    for g4 in range(5):
                        p = ip % 2
                        ip += 1
                        PE.wait(pP_free[p])
                        for j in range(4):
                            tile_i = g4 * 4 + j
                            for dc in range(16):
                                ins = nc.tensor.matmul(pP[p][:, j * 128:(j + 1) * 128], lhsT=hT[:, dc, tile_i * 128:(tile_i + 1) * 128], rhs=wq[s][:, dc, 256:384], start=(dc == 0), stop=(dc == 15))
                        tP = PE.done(ins)
                        src = pP[p][:, :].rearrange("p (a b c) -> p a b c", a=4, b=2)
                        if g4 == 0:
                            t_v = DVE.op(lambda: nc.vector.tensor_scalar(out=va[s][:, 0:4, :, 0:64], in0=src, scalar1=hv, scalar2=None, op0=ALU.mult), tP, prev_att, t_init)
                        else:
                            t_v = ACT.op(lambda: nc.scalar.copy(out=va[s][:, g4 * 4:(g4 + 1) * 4, :, 0:64], in_=src), tP, prev_att, t_init)
                        pP_free[p] = t_v
                    t_vd = DVE.last
                    t_va = ACT.last
                    last_pe = None
                    items = [(cp, h) for cp in range(16) for h in range(2)]

                    def emit_S(i):
                        cp, h = items[i]
                        b = i % 2
                        PE.wait(t_b, t_k, t_q, pSc_free[b], t_const)
                        for blk in range(5):
                            o = pSc[b][:, blk * 128:(blk + 1) * 128]
                            nc.tensor.matmul(o, lhsT=ident_bf[:], rhs=bT[s][:, (h * 5 + blk) * 128:(h * 5 + blk + 1) * 128], start=True, stop=False)
                            ins = nc.tensor.matmul(o, lhsT=kT[s][:, (cp + blk) * 128:(cp + blk + 1) * 128], rhs=qTm[s][:, h, cp * 128:(cp + 1) * 128], start=False, stop=True)
                        tS = PE.done(ins)
                        ACT.op(lambda: nc.scalar.activation(out=pT[b][:, 0:4, :], in_=pSc[b][:, 0:512].rearrange("p (a b) -> p a b", a=4), func=AF.Exp), tS, pT_free[b])
                        t_e2 = ACT.op(lambda: nc.scalar.activation(out=pT[b][:, 4, :], in_=pSc[b][:, 512:640], func=AF.Exp))
                        pSc_free[b] = t_e2
                        return t_e2

                    t_es = {0: emit_S(0)}
                    for i in range(32):
                        cp, h = items[i]
                        b = i % 2
                        ai = cp % 2
                        if i + 1 < 32:
                            t_es[i + 1] = emit_S(i + 1)
                        PE.wait(t_es[i], t_vd, t_va, pO_free[b])
                        for blk in range(5):
                            ins = nc.tensor.matmul(pO[b][:, 0:65], lhsT=pT[b][:, blk, :], rhs=va[s][:, cp + blk, h, :], start=(blk == 0), stop=(blk == 4))
                        tO = PE.done(ins)
                        pT_free[b] = tO
                        t_r = DVE.op(lambda: nc.vector.reciprocal(out=rinv[b][:], in_=pO[b][:, 64:65]), tO)
                        t_a = DVE.op(lambda: nc.vector.tensor_scalar(out=att[ai][:, h * 64:(h + 1) * 64], in0=pO[b][:, 0:64], scalar1=rinv[b][:, 0:1], scalar2=None, op0=ALU.mult), t_r, att_free[ai])
                        pO_free[b] = t_a
                        if h == 1:
                            PE.wait(t_a, pTr_free, pP_free[1])
                            tT = PE.done(nc.tensor.transpose(out=pTr[:, 0:128], in_=att[ai][:], identity=ident_bf[:]))
                            att_free[ai] = tT
                            last_pe = tT
                            pTr_free = ACT.op(lambda: nc.scalar.copy(out=catT[:, 8 + hp, cp * 128:(cp + 1) * 128], in_=pTr[:, 0:128]), tT)
                    wq_free[s] = last_pe
                    prev_att = last_pe
                    pP_free[1] = [pP_free[1], pTr_free]
                barrier()
        if stage == 1:
            s_d = Slot(nc, "dbg", st)
            t = s_d.dma(SP, lambda: nc.sync.dma_start(out=dbg_cat[:, :], in_=catT[:].rearrange("p a b -> p (a b)")))
            SP.wait(t)
            ph_cat.close()
            return nc


        with ExitStack() as ph:
            wo = sb("wo", [128, 16, D], BF16, ph)
            l1 = sb("l1", [128, 2, D], F32, ph)
            hld = [sb(f"hld{i}", [128, D], F32, ph) for i in range(3)]
            h1f = [sb(f"h1f{i}", [128, D], F32, ph) for i in range(2)]
            h1b = [sb(f"h1b{i}", [128, D], BF16, ph) for i in range(2)]
            lnp = [(sb(f"st4{i}", [128, 24], F32, ph), sb(f"mv4{i}", [128, 2], F32, ph), sb(f"sd4{i}", [128, 4], F32, ph)) for i in range(3)]
            pM = [pst(f"pM{i}", [128, 512], F32, ph) for i in range(8)]
            s_wo = Slot(nc, "wo", st)
            s_l1 = Slot(nc, "l1", st)
            s_hl = [Slot(nc, f"hl{i}", st) for i in range(3)]
            s_o1 = [Slot(nc, f"o1{i}", st) for i in range(2)]
            s_o2 = [Slot(nc, f"o2{i}", st) for i in range(2)]
            for dc in range(16):
                t_wo = s_wo.dma(POOL, lambda: nc.gpsimd.dma_start(out=wo[:, dc, :], in_=w_out[dc * 128:(dc + 1) * 128, :]))
            t_l1 = s_l1.dma(SP, lambda: nc.sync.dma_start(out=l1[:, 0, :], in_=ln12[0:1, :].partition_broadcast(128)))
            t_l1 = s_l1.dma(SP, lambda: nc.sync.dma_start(out=l1[:, 1, :], in_=ln12[1:2, :].partition_broadcast(128)))
            hld_free = [None] * 3
            h1f_free = [None, None]
            h1b_free = [None, None]
            pM_free = [None, None]

            def p4_A(tt):
                s = tt % 3
                ps = tt % 2
                rs = slice(tt * 128, (tt + 1) * 128)
                t_h = s_hl[s].dma(SP, lambda: nc.sync.dma_start(out=hld[s][:], in_=hbuf[rs, :]), hld_free[s])
                PE.wait(t_wo, pM_free[ps])
                for nb in range(4):
                    for ec in range(16):
                        ins = nc.tensor.matmul(pM[ps * 4 + nb][:, :], lhsT=catT[:, ec, rs], rhs=wo[:, ec, nb * 512:(nb + 1) * 512], start=(ec == 0), stop=(ec == 15))
                tM = PE.done(ins)
                DVE.wait(t_h, tM)
                for nb in range(4):
                    cs = slice(nb * 512, (nb + 1) * 512)
                    t = DVE.op(lambda: nc.vector.scalar_tensor_tensor(out=hld[s][:, cs], in0=hld[s][:, cs], scalar=ALPHA, in1=pM[ps * 4 + nb][:, :], op0=ALU.mult, op1=ALU.add))
                pM_free[ps] = t
                return t

            t_As = {0: p4_A(0)}
            for tt in range(16):
                s = tt % 3
                hs = tt % 2
                rs = slice(tt * 128, (tt + 1) * 128)
                t_n = ln_norm_act(hld[s], t_As[tt], lnp[s])
                if tt + 1 < 16:
                    t_As[tt + 1] = p4_A(tt + 1)
                t = DVE.op(lambda: nc.vector.tensor_tensor(out=hld[s][:], in0=hld[s][:], in1=l1[:, 0, :], op=ALU.mult), t_n, t_l1)
                t_f = POOL.op(lambda: nc.gpsimd.tensor_tensor(out=h1f[hs][:], in0=hld[s][:], in1=l1[:, 1, :], op=ALU.add), t, h1f_free[hs])
                hld_free[s] = t_f
                t_b16 = ACT.op(lambda: nc.scalar.copy(out=h1b[hs][:], in_=h1f[hs][:]), t_f, h1b_free[hs])
                t_s1 = s_o1[hs].dma(SP, lambda: nc.sync.dma_start(out=h1buf[rs, :], in_=h1f[hs][:]), t_f)
                t_s2 = s_o2[hs].dma(SP, lambda: nc.sync.dma_start(out=h1bf[rs, :], in_=h1b[hs][:]), t_b16)
                h1f_free[hs] = [t_b16, t_s1]
                h1b_free[hs] = t_s2
            barrier()
        ph_cat.close()
        if stage == 2:
            SP.wait([sl.tok() for sl in Slot.allslots])
            return nc

        G_all = sb("G_all", [128, 16, NE], F32)
        selm_bf = sb("selm_bf", [128, 16, NE], BF16)
        selm_f = sb("selm_f", [128, 16, NE], F32)
        slot8_all = sb("slot8_all", [128, 16, 8], I32)
        gate8_all = sb("gate8_all", [128, 16, 8], F32)
        with ExitStack() as ph_moe:
            wgb = [sb(f"wgb{i}", [128, 16, FH], BF16, ph_moe) for i in range(2)]
            wub = [sb(f"wub{i}", [128, 16, FH], BF16, ph_moe) for i in range(2)]
            wdb = [sb(f"wdb{i}", [128, 4, D], BF16, ph_moe) for i in range(2)]
            xg1 = sb("xg1", [128, 4, D], BF16, ph_moe)
            xg = [xg1, xg1]
            xT = [sb(f"xT{i}", [128, 16, CAP], BF16, ph_moe) for i in range(2)]
            hid = sb("hid", [128, 4, CAP], BF16, ph_moe)
            sg = [sb(f"sg{i}", [128, CAP], F32, ph_moe) for i in range(2)]
            yo = [sb(f"yo{i}", [128, D], BF16, ph_moe) for i in range(2)]
            pT2 = pst("pT2", [128, 1024], BF16, ph_moe)
            pGm = pst("pGm", [128, 512], F32, ph_moe)
            pUm = pst("pUm", [128, 512], F32, ph_moe)
            pYm = [pst(f"pYm{i}", [128, 512], F32, ph_moe) for i in range(4)]
            pR = pst("pR", [128, 512], F32, ph_moe)
            s_wgu = [Slot(nc, f"wgu{i}", st) for i in range(2)]
            s_wd = [Slot(nc, f"wd{i}", st) for i in range(2)]
            s_xg = [Slot(nc, f"xg{i}", st) for i in range(2)]
            s_yo = [Slot(nc, f"yo{i}", st) for i in range(2)]
            fst = dict(wgu_free=[None, None], wd_free=[None, None], xg_free=[None, None], xT_free=[None, None], pT2_free=[None, None], itp=0, tp_banks=[pT2[:, :]], pG_free=None, pU_free=None,
                       hid_free=None, sg_free=[None, None], pY_free=[None] * 4, yo_free=[None, None], isg=0, iyo=0)

            def load_w(s, wg_ap, wu_ap, wd_ap):
                t = s_wgu[s].dma(POOL, lambda: nc.gpsimd.dma_start(out=wgb[s][:].rearrange("p c f -> p (c f)"), in_=wg_ap.rearrange("(p c) f -> p (c f)", c=16), max_dma_last_dim=8192), fst["wgu_free"][s])
                t_gu = s_wgu[s].dma(POOL, lambda: nc.gpsimd.dma_start(out=wub[s][:].rearrange("p c f -> p (c f)"), in_=wu_ap.rearrange("(p c) f -> p (c f)", c=16), max_dma_last_dim=8192))
                t_d = s_wd[s].dma(POOL, lambda: nc.gpsimd.dma_start(out=wdb[s][:], in_=wd_ap.rearrange("(c p) n -> p c n", p=128)), fst["wd_free"][s])
                return t_gu, t_d

            def transpose_block(s, ntile, t_x, j0=0):
                for j in range(ntile):
                    for g in range(2):
                        banks = fst["tp_banks"]
                        bi = fst["itp"] % len(banks)
                        fst["itp"] += 1
                        pTb = banks[bi]
                        PE.wait(t_x, fst["pT2_free"][bi], fst["xT_free"][s])
                        for i in range(8):
                            dc = g * 8 + i
                            ins = nc.tensor.transpose(out=pTb[:, i * 128:(i + 1) * 128], in_=xg1[:, j0 + j, dc:D:16], identity=ident_bf[:])
                        tp = PE.done(ins)
                        src = pTb[:, :].rearrange("p (a b) -> p a b", a=8)
                        if g == 0:
                            t = ACT.op(lambda: nc.scalar.copy(out=xT[s][:, 0:8, j * 128:(j + 1) * 128], in_=src), tp, fst["xT_free"][s])
                        else:
                            t = DVE.op(lambda: nc.vector.tensor_copy(out=xT[s][:, 8:16, j * 128:(j + 1) * 128], in_=src), tp, fst["xT_free"][s])
                        fst["pT2_free"][bi] = t
                fst["xg_free"][s] = tp
                return [ACT.last, DVE.last]

            def ffn_gu(s, xs, W, t_w, t_xT):
                for fc in range(4):
                    PE.wait(t_w, t_xT, fst["pG_free"])
                    for dc in range(16):
                        ins = nc.tensor.matmul(pGm[:, 0:W], lhsT=wgb[s][:, dc, fc * 128:(fc + 1) * 128], rhs=xT[xs][:, dc, 0:W], start=(dc == 0), stop=(dc == 15))
                    tG = PE.done(ins)
                    PE.wait(fst["pU_free"])
                    for dc in range(16):
                        ins = nc.tensor.matmul(pUm[:, 0:W], lhsT=wub[s][:, dc, fc * 128:(fc + 1) * 128], rhs=xT[xs][:, dc, 0:W], start=(dc == 0), stop=(dc == 15))
                    tU = PE.done(ins)
                    q = fst["isg"] % 2
                    fst["isg"] += 1
                    t_sg = ACT.op(lambda: nc.scalar.activation(out=sg[q][:, 0:W], in_=pGm[:, 0:W], func=AF.Silu), tG, fst["sg_free"][q])
                    fst["pG_free"] = t_sg
                    t_h = DVE.op(lambda: nc.vector.tensor_tensor(out=hid[:, fc, 0:W], in0=pUm[:, 0:W], in1=sg[q][:, 0:W], op=ALU.mult), tU, t_sg, fst["hid_free"])
                    fst["pU_free"] = t_h
                    fst["sg_free"][q] = t_h
                fst["xT_free"][xs] = tU
                fst["wgu_free"][s] = tU
                return t_h

            def ffn_d(s, W, t_wd, t_h, store, gsc=None):
                last = None
                for j in range((W + 127) // 128):
                    q = fst["iyo"] % 2
                    fst["iyo"] += 1
                    nr = min(128, W - j * 128)
                    tys = []
                    for nb in range(4):
                        PE.wait(t_h, t_wd, fst["pY_free"][nb])
                        for fc in range(4):
                            ins = nc.tensor.matmul(pYm[nb][0:nr, :], lhsT=hid[:, fc, j * 128:j * 128 + nr], rhs=wdb[s][:, fc, nb * 512:(nb + 1) * 512], start=(fc == 0), stop=(fc == 3))
                        tY = PE.done(ins)
                        cs = slice(nb * 512, (nb + 1) * 512)
                        if gsc is not None:
                            g_ap, t_gsc = gsc(j)
                            if nb % 2 == 0:
                                te = ACT.op(lambda: nc.scalar.activation(out=yo[q][0:nr, cs], in_=pYm[nb][0:nr, :], func=AF.Copy, scale=g_ap[0:nr, :]), tY, fst["yo_free"][q], t_gsc)
                            else:
                                te = DVE.op(lambda: nc.vector.tensor_scalar(out=yo[q][0:nr, cs], in0=pYm[nb][0:nr, :], scalar1=g_ap[0:nr, :], scalar2=None, op0=ALU.mult), tY, fst["yo_free"][q], t_gsc)
                        elif nb % 2 == 0:
                            te = ACT.op(lambda: nc.scalar.copy(out=yo[q][0:nr, cs], in_=pYm[nb][0:nr, :]), tY, fst["yo_free"][q])
                        else:
                            te = DVE.op(lambda: nc.vector.tensor_copy(out=yo[q][0:nr, cs], in_=pYm[nb][0:nr, :]), tY, fst["yo_free"][q])
                        fst["pY_free"][nb] = te
                        tys.append(te)
                        last = tY
                    fst["yo_free"][q] = store(j, yo[q], tys, s_yo[q])
                fst["hid_free"] = last
                fst["wd_free"][s] = last

            tokidx_all = sb("tokidx_all", [128, NE * NTL, 4], I32, ph_moe)
            with ExitStack() as ph:
                wr_b = sb("wr_b", [128, 16, NE], BF16, ph)
                rb_row = sb("rb_row", [128, NE], F32, ph)
                sc2 = sb("sc2", [128, 2, NE], F32, ph)
                selv = sb("selv", [128, 2, NE], F32, ph)
                m8 = sb("m8", [128, 2, 8, 8], F32, ph)
                gs = sb("gs", [128, 2, 8], F32, ph)
                gs8 = sb("gs8", [128, 2, 8], F32, ph)
                gm = sb("gm", [128, 2, 8], F32, ph)
                gm1 = sb("gm1", [128, 2, 8], F32, ph)
                msk = sb("msk", [128, 2, NE], F32, ph)
                t8 = sb("t8", [128, 2, 8], F32, ph)
                wv = sb("wv", [128, 2, NE], F32, ph)
                den = sb("den", [128, 2, 2], F32, ph)
                zt = sb("zt", [128, 1028], I32, ph)
                tokrow = sb("tokrow", [128, 16, 4], I32, ph)
                av = sb("av", [128, NE], F32, ph)
                vals = sb("vals", [128, NE], F32, ph)
                s8 = sb("s8", [128, 8], F32, ph)
                s8b = sb("s8b", [128, 8], F32, ph)
                e8 = sb("e8", [128, 8], F32, ph)
                junk = sb("junk", [128, NE], F32, ph)
                dump1 = sb("dump1", [128, 1], F32, ph)
                s_r = Slot(nc, "rt", st)
                s_r2 = Slot(nc, "rt2", st)
                s_z = Slot(nc, "z", st)
                s_sc = Slot(nc, "scat", st)
                s_ti = [Slot(nc, f"tix{i}", st) for i in range(8)]
                t_r = s_r.dma(POOL, lambda: nc.gpsimd.dma_start(out=wr_b[:], in_=w_router.rearrange("(p c) e -> p c e", c=16)))
                t_r = [t_r, s_r2.dma(SP, lambda: nc.sync.dma_start(out=rb_row[:], in_=router_bias[0:1, :].partition_broadcast(128)))]
                t = POOL.op(lambda: nc.gpsimd.memset(zt[:], 0))
                t = POOL.op(lambda: nc.gpsimd.memset(tokrow[:], 0), t)
                stz = slot_tok.rearrange("(p r) c -> p (r c)", p=128)
                t_z = s_z.dma(SP, lambda: nc.sync.dma_start(out=stz[:, :], in_=zt[:]), t)
                t_tr = DVE.op(lambda: nc.vector.tensor_copy(out=tokrow[:, :, 0], in_=cst[:, 512:528]), t)
                t_d1 = DVE.op(lambda: nc.vector.tensor_scalar(out=dump1[:], in0=cst[:, 528:529], scalar1=1.0, scalar2=None, op0=ALU.add))
                t_wgu, t_wd = load_w(0, wsg, wsu, wsd)
                t_ws = {}
                if stage >= 5:
                    t_ws[0] = load_w(1, w_gate[0], w_up[0], w_down[0])

                def lvl(fns, *deps):
                    DVE.wait(*deps)
                    t = None
                    for fn in fns:
                        t = DVE.done(fn())
                    return t

                t_pR_free = None
                t_sc = None
                for tb in range(8):
                    s = tb % 2
                    g0 = tb * 2
                    t_x = s_xg[s].dma(SP, lambda: nc.sync.dma_start(out=xg1[:, 2 * s:2 * s + 2, :], in_=h1bf[tb * 256:(tb + 1) * 256, :].rearrange("(j p) d -> p j d", p=128)), fst["xg_free"][s])
                    t_xT = transpose_block(s, 2, t_x, j0=2 * s)
                    PE.wait(t_xT, t_r, t_pR_free)
                    for j in range(2):
                        for dc in range(16):
                            ins = nc.tensor.matmul(pR[:, j * NE:(j + 1) * NE], lhsT=xT[s][:, dc, j * 128:(j + 1) * 128], rhs=wr_b[:, dc, :], start=(dc == 0), stop=(dc == 15))
                    tR = PE.done(ins)
                    t_sig = ACT.op(lambda: nc.scalar.activation(out=sc2[:].rearrange("p a b -> p (a b)"), in_=pR[:, 0:2 * NE], func=AF.Sigmoid), tR, t_pR_free)
                    t = lvl([lambda j=j: nc.vector.tensor_tensor(out=selv[:, j, :], in0=sc2[:, j, :], in1=rb_row[:], op=ALU.add) for j in range(2)], t_sig, t_r)
                    t = lvl([lambda j=j, g=g: nc.vector.max(out=m8[:, j, g, :], in_=selv[:, j, g * 8:(g + 1) * 8]) for j in range(2) for g in range(8)], t)
                    t = lvl([lambda: nc.vector.tensor_tensor(out=gs[:], in0=m8[:, :, :, 0], in1=m8[:, :, :, 1], op=ALU.add)], t)
                    t = lvl([lambda j=j: nc.vector.max(out=gs8[:, j, :], in_=gs[:, j, :]) for j in range(2)], t)
                    t = lvl([lambda j=j: nc.vector.tensor_scalar(out=gm[:, j, :], in0=gs[:, j, :], scalar1=gs8[:, j, 3:4], scalar2=None, op0=ALU.is_ge) for j in range(2)], t)
                    t = lvl([lambda: nc.vector.tensor_scalar(out=gm1[:], in0=gm[:], scalar1=-1.0, scalar2=None, op0=ALU.add)], t)
                    t = lvl([lambda j=j, g=g: nc.vector.tensor_scalar(out=msk[:, j, g * 8:(g + 1) * 8], in0=selv[:, j, g * 8:(g + 1) * 8], scalar1=gm[:, j, g:g + 1], scalar2=gm1[:, j, g:g + 1], op0=ALU.mult, op1=ALU.add) for j in range(2) for g in range(8)], t)
                    t = lvl([lambda j=j: nc.vector.max(out=t8[:, j, :], in_=msk[:, j, :]) for j in range(2)], t)
                    t = lvl([lambda j=j: nc.vector.tensor_scalar(out=selm_f[:, g0 + j, :], in0=msk[:, j, :], scalar1=t8[:, j, 7:8], scalar2=None, op0=ALU.is_ge) for j in range(2)], t)
                    t_selm = lvl([lambda: nc.vector.tensor_copy(out=selm_bf[:, g0:g0 + 2, :], in_=selm_f[:, g0:g0 + 2, :]),
                                  lambda: nc.vector.tensor_tensor(out=wv[:], in0=sc2[:], in1=selm_f[:, g0:g0 + 2, :], op=ALU.mult)], t)
                    t = lvl([lambda: nc.vector.tensor_reduce(out=den[:, 0, :], in_=wv[:], axis=mybir.AxisListType.X, op=ALU.add)], t_selm)
                    t = lvl([lambda: nc.vector.reciprocal(out=den[:, 1, :], in_=den[:, 0, :])], t)
                    t_G = lvl([lambda j=j: nc.vector.tensor_scalar(out=G_all[:, g0 + j, :], in0=wv[:, j, :], scalar1=den[:, 1, j:j + 1], scalar2=2.5, op0=ALU.mult, op1=ALU.mult) for j in range(2)], t)

                    def store_sh(j, yot, tys, slot, tb=tb):
                        r0 = (tb * 2 + j) * 128
                        return slot.dma(SP, lambda: nc.sync.dma_start(out=yshbuf[r0:r0 + 128, :], in_=yot[:]), tys)
                    t_h = ffn_gu(0, s, 256, t_wgu, t_xT)
                    ffn_d(0, 256, t_wd, t_h, store_sh)
                    PE.wait(t_selm, t_sig)
                    for j in range(2):
                        tt = g0 + j
                        o = pR[:, 128 + j * NE:128 + (j + 1) * NE]
                        ins = nc.tensor.matmul(o, lhsT=lstr_bf[:], rhs=selm_bf[:, tt, :], start=True, stop=(tt == 0))
                        for i in range(tt):
                            ins = nc.tensor.matmul(o, lhsT=ones_bf[:], rhs=selm_bf[:, i, :], start=False, stop=(i == tt - 1))
                    tP = PE.done(ins)
                    t = t_G
                    for j in range(2):
                        tt = g0 + j
                        pos = pR[:, 128 + j * NE:128 + (j + 1) * NE]
                        t = DVE.op(lambda: nc.vector.scalar_tensor_tensor(out=av[:], in0=pos, scalar=float(CAPV), in1=selm_f[:, tt, :], op0=ALU.is_lt, op1=ALU.mult), tP, t)
                        t = DVE.op(lambda: nc.vector.tensor_tensor(out=vals[:], in0=pos, in1=cst[:, 529:593], op=ALU.add), t)
                        t_pR_free = t
                        t = DVE.op(lambda: nc.vector.tensor_tensor(out=vals[:], in0=vals[:], in1=av[:], op=ALU.mult), t)
                        t = DVE.op(lambda: nc.vector.tensor_scalar(out=vals[:], in0=vals[:], scalar1=-1.0, scalar2=None, op0=ALU.add), t)
                        t = DVE.op(lambda: nc.vector.max(out=s8[:], in_=vals[:]), t)
                        t = DVE.op(lambda: nc.vector.tensor_scalar(out=s8b[:], in0=s8[:], scalar1=0.0, scalar2=dump1[:, 0:1], op0=ALU.is_lt, op1=ALU.mult), t, t_d1)
                        t = DVE.op(lambda: nc.vector.tensor_tensor(out=s8b[:], in0=s8b[:], in1=s8[:], op=ALU.add), t)
                        t_s8 = DVE.op(lambda: nc.vector.tensor_copy(out=slot8_all[:, tt, :], in_=s8b[:]), t)
                        t = t_s8
                        for k in range(8):
                            t_sc = s_sc.dma(POOL, lambda: nc.gpsimd.indirect_dma_start(out=slot_tok[:, :], out_offset=bass.IndirectOffsetOnAxis(ap=slot8_all[:, tt, k:k + 1], axis=0), in_=tokrow[:, tt, :], in_offset=None), t_s8, t_z, t_tr)
                if stage >= 5:
                    t_ws[1] = load_w(0, w_gate[1], w_up[1], w_down[1])
                s_gd = Slot(nc, "gd", st)
                t_gd = s_gd.dma(SP, lambda: nc.sync.dma_start(out=gdram.rearrange("(a p) e -> p a e", p=128), in_=G_all[:]), t_G)
                stv = slot_tok[0:NS, :].rearrange("(r p) c -> p r c", p=128)
                t_tis = []
                for i in range(8):
                    t_tis.append(s_ti[i].dma(SP, lambda: nc.sync.dma_start(out=tokidx_all[:, i * 32:(i + 1) * 32, :], in_=stv[:, i * 32:(i + 1) * 32, :]), t_sc))
                barrier(exclude=s_wgu + s_wd + (s_ti if stage >= 5 else []))
            if stage in (3, 4):
                s_d = Slot(nc, "dbg", st)
                t = s_d.dma(SP, lambda: nc.sync.dma_start(out=dbg_gate[:, :], in_=G_all[:].rearrange("p a b -> p (a b)")))
                if stage == 4:
                    t = s_d.dma(SP, lambda: nc.sync.dma_start(out=dbg_s8[:, :], in_=slot8_all[:].rearrange("p a b -> p (a b)")))
                SP.wait(t, [sl.tok() for sl in Slot.allslots])
                return nc

            s_xgp = [Slot(nc, f"xgp{i}", st) for i in range(2)]
            gsl = [sb(f"gsl{i}", [128, NTL, NE], F32, ph_moe) for i in range(2)]
            s_gsl = [Slot(nc, f"gsl{i}", st) for i in range(2)]
            gsl_free = [None, None]
            t_gsl = {}
            fst["tp_banks"] = [pT2[:, :], pR[:, :].bitcast(BF16)]
            fst["itp"] = 0

            def gather_e(e):
                s = e % 2
                t_x = None
                for m in range(NTL):
                    t_x = s_xgp[s].dma(POOL, lambda: nc.gpsimd.indirect_dma_start(out=xg1[:, m, :], out_offset=None, in_=h1bf[:, :], in_offset=bass.IndirectOffsetOnAxis(ap=tokidx_all[:, e * NTL + m, 0:1], axis=0)), (fst["xg_free"][0], fst["xg_free"][1], t_tis[e // 8]) if m == 0 else None)
                for m in range(NTL):
                    t_g = s_gsl[s].dma(POOL, lambda: nc.gpsimd.indirect_dma_start(out=gsl[s][:, m, :], out_offset=None, in_=gdram[:, :], in_offset=bass.IndirectOffsetOnAxis(ap=tokidx_all[:, e * NTL + m, 0:1], axis=0)), (gsl_free[s], t_gd) if m == 0 else None)
                t_gsl[e] = t_g
                return t_x

            t_xg = {0: gather_e(0)}
            t_xTs = {0: transpose_block(0, NTL, t_xg[0])}
            for e in range(NE):
                s = e % 2
                ws = (e + 1) % 2
                if e + 1 < NE:
                    t_xg[e + 1] = gather_e(e + 1)
                    if e + 1 >= 2:
                        t_ws[e + 1] = load_w((e + 2) % 2, w_gate[e + 1], w_up[e + 1], w_down[e + 1])
                t_h = ffn_gu(ws, s, CAPV, t_ws[e][0], t_xTs[e])
                if e + 1 < NE:
                    t_xTs[e + 1] = transpose_block((e + 1) % 2, NTL, t_xg[e + 1])

                def store_e(j, yot, tys, slot, e=e):
                    r0 = e * CAP + j * 128
                    return slot.dma(SP, lambda: nc.sync.dma_start(out=ybuf[r0:r0 + 128, :], in_=yot[:, :]), tys)
                ffn_d(ws, CAPV, t_ws[e][1], t_h, store_e, gsc=lambda j, e=e, s=s: (gsl[s][:, j, e:e + 1], t_gsl[e]))
                gsl_free[s] = [ACT.last, DVE.last]
            barrier()

        with ExitStack() as ph:
            l2 = sb("l2", [128, 2, D], F32, ph)
            acc = [sb(f"acc{i}", [128, D], F32, ph) for i in range(4)]
            ysh = [sb(f"ysh{i}", [128, D], BF16, ph) for i in range(3)]
            yg = [sb(f"yg{i}", [128, D], BF16, ph) for i in range(24)]
            zf = sb("zf", [128, D], BF16, ph)
            dgt = [sb(f"dgt{i}", [128, 16, 128], BF16, ph) for i in range(3)]
            ghl = [sb(f"ghl{i}", [128, 2, 8], F32, ph) for i in range(3)]
            ghb = [sb(f"ghb{i}", [128, 8], BF16, ph) for i in range(3)]
            pC = [pst(f"pC{i}", [128, 512], F32, ph) for i in range(8)]
            lnp = [(sb(f"st7{i}", [128, 24], F32, ph), sb(f"mv7{i}", [128, 2], F32, ph), sb(f"sd7{i}", [128, 4], F32, ph)) for i in range(4)]
            s_l2 = Slot(nc, "l2", st)
            s_zf = Slot(nc, "zf", st)
            s_a = [Slot(nc, f"a{i}", st) for i in range(4)]
            s_ys = [Slot(nc, f"ys{i}", st) for i in range(3)]
            s_yg = [Slot(nc, f"yg{i}", st) for i in range(24)]
            s_out = [Slot(nc, f"out{i}", st) for i in range(4)]
            t_l2 = s_l2.dma(SP, lambda: nc.sync.dma_start(out=l2[:, 0, :], in_=ln12[2:3, :].partition_broadcast(128)))
            t_l2 = s_l2.dma(SP, lambda: nc.sync.dma_start(out=l2[:, 1, :], in_=ln12[3:4, :].partition_broadcast(128)))
            t = POOL.op(lambda: nc.gpsimd.memset(zf[:], 0.0))
            t_zf = s_zf.dma(SP, lambda: nc.sync.dma_start(out=ybuf[NS:NS + 128, :], in_=zf[:]), t)
            acc_free = [None] * 4
            ysh_free = [None] * 3
            yg_free = [None] * 24
            dgt_free = [None] * 3
            pC_free = [None, None]
            tk = {}

            def p7_A0(tt):
                s = tt % 4
                ys = tt % 3
                rs = slice(tt * 128, (tt + 1) * 128)
                t_a = s_a[s].dma(SP, lambda: nc.sync.dma_start(out=acc[s][:], in_=h1buf[rs, :]), acc_free[s])
                t_y = s_ys[ys].dma(SP, lambda: nc.sync.dma_start(out=ysh[ys][:], in_=yshbuf[rs, :]), ysh_free[ys])
                t_gs = []
                for k in range(8):
                    q = ys * 8 + k
                    t_gs.append(s_yg[q].dma(POOL, lambda: nc.gpsimd.indirect_dma_start(out=yg[q][:], out_offset=None, in_=ybuf[:, :], in_offset=bass.IndirectOffsetOnAxis(ap=slot8_all[:, tt, k:k + 1], axis=0)), yg_free[q], t_zf))
                t_dg = None
                t_base = DVE.op(lambda: nc.vector.scalar_tensor_tensor(out=acc[s][:], in0=acc[s][:], scalar=ALPHA, in1=ysh[ys][:], op0=ALU.mult, op1=ALU.add), t_a, t_y)
                ysh_free[ys] = t_base
                tk[tt] = (t_gs, t_dg, t_base)

            def p7_A1(tt):
                s = tt % 4
                ys = tt % 3
                ps = tt % 2
                t_gs, t_dg, t_base = tk[tt]
                PE.wait(t_dg, pC_free[ps])
                for k in range(8):
                    PE.wait(t_gs[k])
                    for nb in range(4):
                        ins = nc.tensor.matmul(pC[ps * 4 + nb][:, :], lhsT=ident_bf[:], rhs=yg[ys * 8 + k][:, nb * 512:(nb + 1) * 512], start=(k == 0), stop=(k == 7))
                tC = PE.done(ins)
                for k in range(8):
                    yg_free[ys * 8 + k] = tC
                dgt_free[ys] = tC
                DVE.wait(tC, t_base)
                for nb in range(4):
                    cs = slice(nb * 512, (nb + 1) * 512)
                    t = DVE.done(nc.vector.tensor_tensor(out=acc[s][:, cs], in0=acc[s][:, cs], in1=pC[ps * 4 + nb][:, :], op=ALU.add))
                pC_free[ps] = t
                return t

            p7_A0(0)
            p7_A0(1)
            t_As = {0: p7_A1(0)}
            for tt in range(16):
                s = tt % 4
                rs = slice(tt * 128, (tt + 1) * 128)
                if tt + 2 < 16:
                    p7_A0(tt + 2)
                t_n = ln_norm_act(acc[s], t_As[tt], lnp[s])
                t = DVE.op(lambda: nc.vector.tensor_tensor(out=acc[s][:], in0=acc[s][:], in1=l2[:, 0, :], op=ALU.mult), t_n, t_l2)
                t = DVE.op(lambda: nc.vector.tensor_tensor(out=acc[s][:], in0=acc[s][:], in1=l2[:, 1, :], op=ALU.add), t)
                acc_free[s] = s_out[s].dma(ACT, lambda: nc.scalar.dma_start(out=y[rs, :], in_=acc[s][:]), t)
                if tt + 1 < 16:
                    t_As[tt + 1] = p7_A1(tt + 1)
            SP.wait([sl.tok() for sl in Slot.allslots])
    return nc


def prep_inputs(inp, stage=99):
    f = np.float32
    x = np.asarray(inp["x"], f)
    common = {}
    cst = np.zeros((128, NCONST), f)
    cst[:, 0:128] = np.eye(128, dtype=f)
    cst[:, 128:256] = np.triu(np.ones((128, 128), f), 1)
    cst[:, 256:384] = 1.0
    cst[:, 384:448] = np.arange(64, dtype=f)[None, :]
    cst[:, 448:512] = (np.arange(64, dtype=f) * CAP)[None, :]
    cst[:, 512:528] = np.arange(16, dtype=f)[None, :] * 128 + np.arange(128, dtype=f)[:, None]
    cst[:, 528] = NS + np.arange(128, dtype=f)
    cst[:, 529:529 + 64] = np.arange(64, dtype=f)[None, :] * CAP + 1.0
    common["consts"] = cst
    common["ln_in_gb"] = np.stack([np.asarray(inp["ln_in_g"], f), np.asarray(inp["ln_in_b"], f)])
    common["w_in"] = np.ascontiguousarray(np.asarray(inp["w_in"], f)[0])
    cw = np.asarray(inp["conv_w"], f)[0]
    common["conv_wT"] = np.ascontiguousarray(cw.T.reshape(8, 128, 31).transpose(1, 0, 2).reshape(128, 8 * 31))
    cols = lambda v: np.asarray(v, f)[0].reshape(8, 128).T
    common["conv_par"] = np.ascontiguousarray(np.concatenate([cols(inp["conv_b"]), cols(inp["conv_ln_g"]), cols(inp["conv_ln_b"])], axis=1))
    rb = np.asarray(inp["rel_bias"], f)[0]
    qi = np.arange(128)
    kk = np.arange(640)
    rel = 512 + qi[None, :] - kk[:, None]
    idx = np.clip(rel, -63, 256) + 63
    bias = rb[:, idx]
    kloc = kk[:, None] - 64 * (qi[None, :] >= 64)
    valid = (kloc >= 0) & (kloc < 576)
    bias = np.where(valid[None], bias, f(-30000.0)).astype(f)
    common["biasT"] = np.ascontiguousarray(bias.reshape(16, 5, 128, 128).transpose(2, 0, 1, 3).reshape(128, 16 * 5 * 128))
    common["w_out"] = np.ascontiguousarray(np.asarray(inp["w_out"], f)[0])
    common["ln12"] = np.stack([np.asarray(inp[k], f)[0] for k in ("ln1_g", "ln1_b", "ln2_g", "ln2_b")])
    common["w_router"] = np.ascontiguousarray(np.asarray(inp["w_router"], f)[0])
    common["router_bias"] = np.asarray(inp["router_bias"], f).reshape(1, NE)
    if stage >= 5:
        common["w_gate"] = np.asarray(inp["w_gate"], f)[0]
        common["w_up"] = np.asarray(inp["w_up"], f)[0]
        common["w_down"] = np.asarray(inp["w_down"], f)[0]
    common["wsg"] = np.asarray(inp["w_shared_gate"], f)[0]
    common["wsu"] = np.asarray(inp["w_shared_up"], f)[0]
    common["wsd"] = np.asarray(inp["w_shared_down"], f)[0]
    maps = []
    for c in range(8):
        b, half = c // 2, c % 2
        xs = np.zeros((NTH, D), f)
        if half == 1:
            xs[:] = x[b, NT - HALO:2 * NT]
        else:
            xs[HALO:] = x[b, 0:NT]
        m = dict(common)
        m["x"] = xs
        m["flags"] = np.full((128, 2), float(half), f)
        maps.append(m)
    return maps


_NC_CACHE = {}


def kernel(**inputs):
    maps = prep_inputs(inputs)
    if "nc" not in _NC_CACHE:
        _NC_CACHE["nc"] = build_nc()
    res = run_bass_kernel_spmd(_NC_CACHE["nc"], maps, core_ids=list(range(8)))
    out = np.zeros((4, 2 * NT, D), np.float32)
    for c in range(8):
        b, half = c // 2, c % 2
        out[b, half * NT:(half + 1) * NT] = res.results[c]["y"]
    return out
```

```python
import numpy as np
from contextlib import ExitStack
import concourse.bass as bass
import concourse.mybir as mybir
from concourse.bass_utils import run_bass_kernel_spmd

F32, BF16, I32 = mybir.dt.float32, mybir.dt.bfloat16, mybir.dt.int32
AF = mybir.ActivationFunctionType
ALU = mybir.AluOpType

D = 2048
NT = 2048
HALO = 512
NTH = NT + HALO
NE = 64
CAP = 512
CAPV = 448
NTL = CAP // 128
NS = NE * CAP
FH = 512
ALPHA = 2.0 ** 0.25
EPS = 1e-5
NCONST = 640


class Tok:
    __slots__ = ("sem", "val")

    def __init__(self, sem, val):
        self.sem = sem
        self.val = val


def _flat(ts):
    for t in ts:
        if t is None:
            continue
        if isinstance(t, (list, tuple)):
            yield from _flat(t)
        else:
            yield t


class Eng:
    def __init__(self, nc, h, name, st):
        self.h = h
        self.sem = st.enter_context(nc.semaphore("s_" + name))
        self.n = 0
        self.seen = {}
        self.last = None

    def wait(self, *toks):
        for t in _flat(toks):
            k = id(t.sem)
            if self.seen.get(k, 0) < t.val:
                self.h.wait_ge(t.sem, t.val)
                self.seen[k] = t.val

    def done(self, ins):
        self.n += 1
        ins.then_inc(self.sem, 1)
        self.last = Tok(self.sem, self.n)
        return self.last

    def op(self, fn, *deps):
        self.wait(*deps)
        return self.done(fn())

    def raw(self, fn, *deps):
        self.wait(*deps)
        fn()


class Slot:
    allslots = []

    def __init__(self, nc, name, st):
        self.sem = st.enter_context(nc.semaphore("d_" + name))
        self.cnt = 0
        Slot.allslots.append(self)

    def dma(self, q, fn, *deps):
        q.wait(*deps)
        self.cnt += 16
        fn().then_inc(self.sem, 16)
        return Tok(self.sem, self.cnt)

    def tok(self):
        return Tok(self.sem, self.cnt) if self.cnt else None


def build_nc(stage=99):
    Slot.allslots = []
    nc = bass.Bass("TRN2", target_bir_lowering=False)

    def din(name, shape, dt=F32):
        return nc.dram_tensor(name, shape, dt, kind="ExternalInput").ap()

    x = din("x", [NTH, D])
    flags = din("flags", [128, 2])
    consts = din("consts", [128, NCONST])
    ln_in_gb = din("ln_in_gb", [2, D])
    w_in = din("w_in", [D, 5120])
    conv_wT = din("conv_wT", [128, 8 * 31])
    conv_par = din("conv_par", [128, 24])
    biasT = din("biasT", [128, 16 * 5 * 128])
    w_out = din("w_out", [D, D])
    ln12 = din("ln12", [4, D])
    w_router = din("w_router", [D, NE])
    router_bias = din("router_bias", [1, NE])
    if stage >= 5:
        w_gate = din("w_gate", [NE, D, FH])
        w_up = din("w_up", [NE, D, FH])
        w_down = din("w_down", [NE, FH, D])
    wsg = din("wsg", [D, FH])
    wsu = din("wsu", [D, FH])
    wsd = din("wsd", [FH, D])
    y = nc.dram_tensor("y", [NT, D], F32, kind="ExternalOutput").ap()

    def dscr(name, shape, dt, dbg=False):
        return nc.dram_tensor(name, shape, dt, kind="ExternalOutput" if dbg else "Internal").ap()

    hbuf = dscr("hbuf", [NT, D], F32)
    h1buf = dscr("h1buf", [NT, D], F32, dbg=(stage == 2))
    h1bf = dscr("h1bf", [NT, D], BF16)
    yshbuf = dscr("yshbuf", [NT, D], BF16, dbg=(stage == 3))
    slot_tok = dscr("slot_tok", [NS + 128, 4], I32, dbg=(stage == 4))
    ybuf = dscr("ybuf", [NS + 128, D], BF16)
    dbg_cat = dscr("dbg_cat", [128, 16 * NT], BF16, dbg=(stage == 1)) if stage == 1 else None
    dbg_gate = dscr("dbg_gate", [128, 16 * 64], F32, dbg=True) if stage in (3, 4) else None
    dbg_s8 = dscr("dbg_s8", [128, 16 * 8], I32, dbg=True) if stage == 4 else None

    with ExitStack() as st:
        PE = Eng(nc, nc.tensor, "pe", st)
        ACT = Eng(nc, nc.scalar, "act", st)
        DVE = Eng(nc, nc.vector, "dve", st)
        POOL = Eng(nc, nc.gpsimd, "pool", st)
        SP = Eng(nc, nc.sync, "sp", st)
        engines = [PE, ACT, DVE, POOL, SP]

        def sb(name, shape, dt, stack=st):
            return stack.enter_context(nc.sbuf_tensor(name, shape, dt))

        def pst(name, shape, dt, stack):
            return stack.enter_context(nc.psum_tensor(name, shape, dt))

        def barrier(exclude=()):
            toks = [e.last for e in engines] + [s.tok() for s in Slot.allslots if s not in exclude]
            for e in engines:
                e.wait(toks)

        cst = sb("cst", [128, NCONST], F32)
        ident_bf = sb("ident_bf", [128, 128], BF16)
        lstr_bf = sb("lstr_bf", [128, 128], BF16)
        ones_bf = sb("ones_bf", [128, 128], BF16)
        flg = sb("flg", [128, 2], F32)
        eps_t = sb("eps_t", [128, 1], F32)
        ph_cat = ExitStack()
        catT = sb("catT", [128, 16, NT], BF16, ph_cat)
        s_c = Slot(nc, "c", st)
        t_c = s_c.dma(SP, lambda: nc.sync.dma_start(out=cst[:], in_=consts[:, :]))
        t_c = s_c.dma(SP, lambda: nc.sync.dma_start(out=flg[:], in_=flags[:, :]))
        t0 = DVE.op(lambda: nc.vector.tensor_copy(out=ident_bf[:], in_=cst[:, 0:128]), t_c)
        t0 = DVE.op(lambda: nc.vector.tensor_copy(out=lstr_bf[:], in_=cst[:, 128:256]))
        t0 = DVE.op(lambda: nc.vector.tensor_copy(out=ones_bf[:], in_=cst[:, 256:384]))
        t_const = DVE.op(lambda: nc.vector.memset(eps_t[:], EPS))
        ones_f = cst[:, 256:384]
        hv = flg[:, 0:1]

        def ln_stats(xt, n, tok_in, pool):
            stats, mv, sd = pool
            DVE.wait(tok_in)
            nchunk = n // 512
            for j in range(nchunk):
                t = DVE.op(lambda j=j: nc.vector.bn_stats(out=stats[:, j * 6:(j + 1) * 6], in_=xt[:, j * 512:(j + 1) * 512]))
            t = DVE.op(lambda: nc.vector.bn_aggr(out=mv[:, 0:2], in_=stats[:, 0:6 * nchunk]), t)
            t = ACT.op(lambda: nc.scalar.activation(out=sd[:, 0:1], in_=mv[:, 1:2], func=AF.Sqrt, bias=eps_t[:, 0:1], scale=1.0), t, t_const)
            t = DVE.op(lambda: nc.vector.reciprocal(out=sd[:, 1:2], in_=sd[:, 0:1]), t)
            return mv[:, 0:1], sd[:, 1:2], t

        def ln_norm_act(buf, tok_in, pool):
            mean, rstd, t = ln_stats(buf, D, tok_in, pool)
            sd = pool[2]
            t = DVE.op(lambda: nc.vector.tensor_scalar(out=sd[:, 2:3], in0=mean, scalar1=rstd, scalar2=-1.0, op0=ALU.mult, op1=ALU.mult), t)
            return ACT.op(lambda: nc.scalar.activation(out=buf[:], in_=buf[:], func=AF.Identity, bias=sd[:, 2:3], scale=rstd), t)

        with ExitStack() as ph_mix:
            hT = sb("hT", [128, 16, NTH], BF16, ph_mix)
            with ExitStack() as ph:
                gbrow = sb("gbrow", [128, 2, D], F32, ph)
                xt = [sb(f"xt{i}", [128, D], F32, ph) for i in range(2)]
                hf = [sb(f"hf{i}", [128, D], F32, ph) for i in range(2)]
                hb = [sb(f"hb{i}", [128, D], BF16, ph) for i in range(2)]
                lnp = [(sb(f"st{i}", [128, 24], F32, ph), sb(f"mv{i}", [128, 2], F32, ph), sb(f"sd{i}", [128, 2], F32, ph)) for i in range(2)]
                ptr = [pst(f"ptr{i}", [128, 1024], BF16, ph) for i in range(4)]
                s_x = [Slot(nc, f"x{i}", st) for i in range(2)]
                s_h = [Slot(nc, f"h{i}", st) for i in range(2)]
                s_g = Slot(nc, "gb", st)
                t_g = s_g.dma(SP, lambda: nc.sync.dma_start(out=gbrow[:, 0, :], in_=ln_in_gb[0:1, :].partition_broadcast(128)))
                t_g = s_g.dma(SP, lambda: nc.sync.dma_start(out=gbrow[:, 1, :], in_=ln_in_gb[1:2, :].partition_broadcast(128)))
                x_free = [None, None]
                hf_free = [None, None]
                hb_free = [None, None]
                ptr_free = [None] * 4
                NTI = NTH // 128
                t_hfs = [None] * NTI
                for it in range(NTI + 1):
                    if it < NTI:
                        tt = it
                        s = tt % 2
                        t_ld = s_x[s].dma(SP, lambda: nc.sync.dma_start(out=xt[s][:], in_=x[tt * 128:(tt + 1) * 128, :]), x_free[s])
                        mean, rstd, t = ln_stats(xt[s], D, t_ld, lnp[s])
                        t = DVE.op(lambda: nc.vector.tensor_scalar(out=hf[s][:], in0=xt[s][:], scalar1=mean, scalar2=rstd, op0=ALU.subtract, op1=ALU.mult), t, hf_free[s])
                        x_free[s] = t
                        t = DVE.op(lambda: nc.vector.tensor_tensor(out=hf[s][:], in0=hf[s][:], in1=gbrow[:, 0, :], op=ALU.mult), t, t_g)
                        t_hfs[tt] = POOL.op(lambda: nc.gpsimd.tensor_tensor(out=hf[s][:], in0=hf[s][:], in1=gbrow[:, 1, :], op=ALU.add), t)
                    if it >= 1:
                        tt = it - 1
                        s = tt % 2
                        t_hf = t_hfs[tt]
                        t_hb = ACT.op(lambda: nc.scalar.copy(out=hb[s][:], in_=hf[s][:]), t_hf, hb_free[s])
                        if tt >= HALO // 128:
                            r0 = (tt - HALO // 128) * 128
                            t_st = s_h[s].dma(SP, lambda: nc.sync.dma_start(out=hbuf[r0:r0 + 128, :], in_=hf[s][:]), t_hf)
                            hf_free[s] = [t_hb, t_st]
                        else:
                            hf_free[s] = t_hb
                        tps = []
                        for g in range(2):
                            pi = (tt % 2) * 2 + g
                            PE.wait(t_hb, ptr_free[pi])
                            for j in range(8):
                                dc = g * 8 + j
                                ins = nc.tensor.transpose(out=ptr[pi][:, j * 128:(j + 1) * 128], in_=hb[s][:, dc * 128:(dc + 1) * 128], identity=ident_bf[:])
                            tp = PE.done(ins)
                            if g == 0:
                                te = ACT.op(lambda: nc.scalar.copy(out=hT[:, 0:8, tt * 128:(tt + 1) * 128], in_=ptr[pi][:, :].rearrange("p (a b) -> p a b", a=8)), tp)
                            else:
                                te = DVE.op(lambda: nc.vector.tensor_copy(out=hT[:, 8:16, tt * 128:(tt + 1) * 128], in_=ptr[pi][:, :].rearrange("p (a b) -> p a b", a=8)), tp)
                            ptr_free[pi] = te
                            tps.append(tp)
                        hb_free[s] = tps
                barrier()

            with ExitStack() as ph:
                wc = [sb(f"wc{i}", [128, 16, 256], BF16, ph) for i in range(2)]
                dg = [sb(f"dg{i}", [128, 31, 128], BF16, ph) for i in range(2)]
                vT = [sb(f"vT{i}", [128, 32 + NT], BF16, ph) for i in range(2)]
                cw = sb("cw", [128, 8 * 31], F32, ph)
                cpar = sb("cpar", [128, 24], F32, ph)
                sig = [sb(f"sig{i}", [128, 512], F32, ph) for i in range(2)]
                pA = [pst(f"pA{i}", [128, 512], F32, ph) for i in range(2)]
                pG = [pst(f"pG{i}", [128, 512], F32, ph) for i in range(2)]
                pY = [pst(f"pY{i}", [128, 512], F32, ph) for i in range(2)]
                pS = [pst(f"pS{i}", [128, 512], F32, ph) for i in range(2)]
                s_w = [Slot(nc, f"wc{i}", st) for i in range(2)]
                s_p = Slot(nc, "cpar", st)
                t_p = s_p.dma(SP, lambda: nc.sync.dma_start(out=cw[:], in_=conv_wT[:, :]))
                t_p = s_p.dma(SP, lambda: nc.sync.dma_start(out=cpar[:], in_=conv_par[:, :]))
                w_in_v = w_in.rearrange("(c p) n -> p c n", p=128)
                wc_free = [None, None]
                dg_free = [None, None]
                vT_free = [None, None]
                pA_free = [None, None]
                pG_free = [None, None]
                pY_free = [None, None]
                sig_free = [None, None]
                ia = 0
                iy = 0
                for ct in range(8):
                    s = ct % 2
                    t_w = s_w[s].dma(POOL, lambda: nc.gpsimd.dma_start(out=wc[s][:, :, 0:128], in_=w_in_v[:, :, ct * 128:(ct + 1) * 128]), wc_free[s])
                    t_w = s_w[s].dma(POOL, lambda: nc.gpsimd.dma_start(out=wc[s][:, :, 128:256], in_=w_in_v[:, :, 1024 + ct * 128:1024 + (ct + 1) * 128]))
                    POOL.wait(t_p, dg_free[s], t_const)
                    for k in range(31):
                        t_dg = POOL.op(lambda k=k: nc.gpsimd.tensor_scalar(out=dg[s][:, k, :], in0=ident_bf[:], scalar1=cw[:, ct * 31 + k:ct * 31 + k + 1], scalar2=1.0, op0=ALU.mult, op1=ALU.mult))
                    t_v_prev = None
                    last_pe = None
                    for tb in range(-1, 4):
                        if tb < 0:
                            n, c0 = 32, HALO - 32
                        else:
                            n, c0 = 512, HALO + tb * 512
                        a = ia % 2
                        ia += 1
                        PE.wait(t_w, pA_free[a])
                        for dc in range(16):
                            ins = nc.tensor.matmul(pA[a][:, 0:n], lhsT=wc[s][:, dc, 0:128], rhs=hT[:, dc, c0:c0 + n], start=(dc == 0), stop=(dc == 15))
                        tA = PE.done(ins)
                        PE.wait(pG_free[a])
                        for dc in range(16):
                            ins = nc.tensor.matmul(pG[a][:, 0:n], lhsT=wc[s][:, dc, 128:256], rhs=hT[:, dc, c0:c0 + n], start=(dc == 0), stop=(dc == 15))
                        tG = PE.done(ins)
                        t_sig = ACT.op(lambda: nc.scalar.activation(out=sig[a][:, 0:n], in_=pG[a][:, 0:n], func=AF.Sigmoid), tG, sig_free[a])
                        pG_free[a] = t_sig
                        if tb < 0:
                            t_v = DVE.op(lambda: nc.vector.scalar_tensor_tensor(out=vT[s][:, 0:32], in0=pA[a][:, 0:32], scalar=hv, in1=sig[a][:, 0:32], op0=ALU.mult, op1=ALU.mult), tA, t_sig, vT_free[s])
                        else:
                            t_v = DVE.op(lambda: nc.vector.tensor_tensor(out=vT[s][:, 32 + tb * 512:32 + (tb + 1) * 512], in0=pA[a][:, :], in1=sig[a][:, :], op=ALU.mult), tA, t_sig, vT_free[s])
                        pA_free[a] = t_v
                        sig_free[a] = t_v
                        if tb >= 0:
                            yb = iy % 2
                            iy += 1
                            PE.wait(t_v, t_v_prev, t_dg, pY_free[yb])
                            for k in range(31):
                                ins = nc.tensor.matmul(pY[yb][:, :], lhsT=dg[s][:, k, :], rhs=vT[s][:, 2 + tb * 512 + k:2 + tb * 512 + k + 512], start=(k == 0), stop=(k == 30))
                            tY = PE.done(ins)
                            last_pe = tY
                            t_e = ACT.op(lambda: nc.scalar.activation(out=catT[:, ct, tb * 512:(tb + 1) * 512], in_=pY[yb][:, :], func=AF.Identity, bias=cpar[:, ct:ct + 1], scale=1.0), tY, t_p)
                            pY_free[yb] = t_e
                        t_v_prev = t_v
                    wc_free[s] = last_pe
                    dg_free[s] = last_pe
                    vT_free[s] = last_pe
                barrier()
                sq = [sb(f"sq{i}", [128, 512], F32, ph) for i in range(2)]
                mean_s = sb("mean_s", [128, 512], F32, ph)
                rstd_s = sb("rstd_s", [128, 512], F32, ph)
                tmpv = sb("tmpv", [128, 512], F32, ph)
                zt = [sb(f"zt{i}", [128, 512], F32, ph) for i in range(2)]
                sq_free = [None, None]
                zt_free = [None, None]
                t_prev_norm = None
                for tb in range(4):
                    cs = slice(tb * 512, (tb + 1) * 512)
                    PE.wait(t_prev_norm)
                    for ct in range(8):
                        ins = nc.tensor.matmul(pS[0][:, :], lhsT=ones_bf[:], rhs=catT[:, ct, cs], start=(ct == 0), stop=(ct == 7))
                    tS1 = PE.done(ins)
                    for ct in range(8):
                        q = ct % 2
                        t_sq = DVE.op(lambda: nc.vector.tensor_tensor(out=sq[q][:], in0=catT[:, ct, cs], in1=catT[:, ct, cs], op=ALU.mult), sq_free[q])
                        PE.wait(t_sq)
                        ins = nc.tensor.matmul(pS[1][:, :], lhsT=ones_f, rhs=sq[q][:], start=(ct == 0), stop=(ct == 7))
                        sq_free[q] = PE.done(ins)
                    tS2 = sq_free[1]
                    t = DVE.op(lambda: nc.vector.tensor_scalar(out=mean_s[:], in0=pS[0][:, :], scalar1=1.0 / 1024, scalar2=None, op0=ALU.mult), tS1, t_prev_norm)
                    t = DVE.op(lambda: nc.vector.tensor_tensor(out=tmpv[:], in0=mean_s[:], in1=mean_s[:], op=ALU.mult), t)
                    t = DVE.op(lambda: nc.vector.scalar_tensor_tensor(out=tmpv[:], in0=pS[1][:, :], scalar=1.0 / 1024, in1=tmpv[:], op0=ALU.mult, op1=ALU.subtract), t, tS2)
                    t = ACT.op(lambda: nc.scalar.activation(out=tmpv[:], in_=tmpv[:], func=AF.Sqrt, bias=eps_t[:, 0:1], scale=1.0), t)
                    t_r = DVE.op(lambda: nc.vector.reciprocal(out=rstd_s[:], in_=tmpv[:]), t)
                    for ct in range(8):
                        q = ct % 2
                        t = DVE.op(lambda: nc.vector.tensor_tensor(out=zt[q][:], in0=catT[:, ct, cs], in1=mean_s[:], op=ALU.subtract), t_r, zt_free[q])
                        t = DVE.op(lambda: nc.vector.tensor_tensor(out=zt[q][:], in0=zt[q][:], in1=rstd_s[:], op=ALU.mult), t)
                        t = ACT.op(lambda: nc.scalar.activation(out=catT[:, ct, cs], in_=zt[q][:], func=AF.Silu, bias=cpar[:, 16 + ct:17 + ct], scale=cpar[:, 8 + ct:9 + ct]), t)
                        zt_free[q] = t
                    t_prev_norm = t
                barrier()

            with ExitStack() as ph:
                bT = [sb(f"bT{i}", [128, 1280], BF16, ph) for i in range(2)]
                wq = [sb(f"wq{i}", [128, 16, 384], BF16, ph) for i in range(2)]
                kT = [sb(f"kT{i}", [128, NTH], BF16, ph) for i in range(1)] * 2
                qTm = [sb(f"qTm{i}", [128, 2, NT], BF16, ph) for i in range(1)] * 2
                va = [sb(f"va{i}", [128, 20, 2, 65], BF16, ph) for i in range(1)] * 2
                pT = [sb(f"pT{i}", [128, 5, 128], BF16, ph) for i in range(2)]
                att = [sb(f"att{i}", [128, 128], BF16, ph) for i in range(2)]
                rinv = [sb(f"rinv{i}", [128, 1], F32, ph) for i in range(2)]

                pSc = [pst(f"pSc{i}", [128, 1024], F32, ph) for i in range(2)]
                pO = [pst(f"pO{i}", [128, 512], F32, ph) for i in range(2)]
                pP = [pst(f"pP{i}", [128, 512], F32, ph) for i in range(2)]
                pTr = pP[1][:, :].bitcast(BF16)
                s_b = [Slot(nc, f"bT{i}", st) for i in range(2)]
                s_wq = [Slot(nc, f"wq{i}", st) for i in range(2)]
                t_init = None
                for i in range(1):
                    t_init = POOL.op(lambda: nc.gpsimd.memset(qTm[i][64:128, 0, :], 0.0))
                    t_init = POOL.op(lambda: nc.gpsimd.memset(qTm[i][0:64, 1, :], 0.0))
                    t_init = POOL.op(lambda: nc.gpsimd.memset(va[i][:, 4:20, :, 64:65], 1.0))
                    t_init = POOL.op(lambda: nc.gpsimd.tensor_scalar(out=va[i][:, 0:4, :, 64:65], in0=va[i][:, 4:8, :, 64:65], scalar1=hv, scalar2=1.0, op0=ALU.mult, op1=ALU.mult), t_init)
                wq_free = [None, None]
                pP_free = [None, None]
                pSc_free = [None, None]
                pT_free = [None, None]
                pO_free = [None, None]
                att_free = [None, None]
                pTr_free = None
                ip = 0
                isc = 0
                iatt = 0
                prev_att = None
                for hp in range(8):
                    s = hp % 2
                    t_b = s_b[s].dma(POOL, lambda: nc.gpsimd.dma_start(out=bT[s][:, :], in_=biasT[:, hp * 1280:(hp + 1) * 1280]), wq_free[s])
                    for j in range(3):
                        c0 = 2048 + j * 1024 + hp * 128
                        t_w = s_wq[s].dma(POOL, lambda: nc.gpsimd.dma_start(out=wq[s][:, :, j * 128:(j + 1) * 128], in_=w_in_v[:, :, c0:c0 + 128]), wq_free[s] if j == 0 else None)
                    for blk in range(5):
                        p = ip % 2
                        ip += 1
                        PE.wait(t_w, pP_free[p])
                        for dc in range(16):
                            ins = nc.tensor.matmul(pP[p][:, :], lhsT=wq[s][:, dc, 128:256], rhs=hT[:, dc, blk * 512:(blk + 1) * 512], start=(dc == 0), stop=(dc == 15))
                        tP = PE.done(ins)
                        pP_free[p] = ACT.op(lambda: nc.scalar.copy(out=kT[s][:, blk * 512:(blk + 1) * 512], in_=pP[p][:, :]), tP, prev_att)
                    t_k = ACT.last
                    for blk in range(4):
                        p = ip % 2
                        ip += 1
                        PE.wait(pP_free[p])
                        for dc in range(16):
                            ins = nc.tensor.matmul(pP[p][:, :], lhsT=wq[s][:, dc, 0:128], rhs=hT[:, dc, HALO + blk * 512:HALO + (blk + 1) * 512], start=(dc == 0), stop=(dc == 15))
                        tP = PE.done(ins)
                        t1 = DVE.op(lambda: nc.vector.tensor_scalar(out=qTm[s][0:64, 0, blk * 512:(blk + 1) * 512], in0=pP[p][0:64, :], scalar1=0.125, scalar2=None, op0=ALU.mult), tP, prev_att, t_init)
                        t2 = DVE.op(lambda: nc.vector.tensor_scalar(out=qTm[s][64:128, 1, blk * 512:(blk + 1) * 512], in0=pP[p][64:128, :], scalar1=0.125, scalar2=None, op0=ALU.mult))
                        pP_free[p] = t2
                    t_q = t2
                    for g4 in range(5):
                        p = ip % 2
                        ip += 1
                        PE.wait(pP_free[p])
                        for j in range(4):
                            tile_i = g4 * 4 + j
                            for dc in range(16):
                                ins = nc.tensor.matmul(pP[p][:, j * 128:(j + 1) * 128], lhsT=hT[:, dc, tile_i * 128:(tile_i + 1) * 128], rhs=wq[s][:, dc, 256:384], start=(dc == 0), stop=(dc == 15))
                        tP = PE.done(ins)
                        src = pP[p][:, :].rearrange("p (a b c) -> p a b c", a=4, b=2)
                        if g4 == 0:
                            t_v = DVE.op(lambda: nc.vector.tensor_scalar(out=va[s][:, 0:4, :, 0:64], in0=src, scalar1=hv, scalar2=None, op0=ALU.mult), tP, prev_att, t_init)
                        else:
                            t_v = ACT.op(lambda: nc.scalar.copy(out=va[s][:, g4 * 4:(g4 + 1) * 4, :, 0:64], in_=src), tP, prev_att, t_init)
                        pP_free[p] = t_v
                    t_vd = DVE.last
                    t_va = ACT.last
                    last_pe = None
                    items = [(cp, h) for cp in range(16) for h in range(2)]

                    def emit_S(i):
                        cp, h = items[i]
                        b = i % 2
                        PE.wait(t_b, t_k, t_q, pSc_free[b], t_const)
                        for blk in range(5):
                            o = pSc[b][:, blk * 128:(blk + 1) * 128]
                            nc.tensor.matmul(o, lhsT=ident_bf[:], rhs=bT[s][:, (h * 5 + blk) * 128:(h * 5 + blk + 1) * 128], start=True, stop=False)
                            ins = nc.tensor.matmul(o, lhsT=kT[s][:, (cp + blk) * 128:(cp + blk + 1) * 128], rhs=qTm[s][:, h, cp * 128:(cp + 1) * 128], start=False, stop=True)
                        tS = PE.done(ins)
                        ACT.op(lambda: nc.scalar.activation(out=pT[b][:, 0:4, :], in_=pSc[b][:, 0:512].rearrange("p (a b) -> p a b", a=4), func=AF.Exp), tS, pT_free[b])
                        t_e2 = ACT.op(lambda: nc.scalar.activation(out=pT[b][:, 4, :], in_=pSc[b][:, 512:640], func=AF.Exp))
                        pSc_free[b] = t_e2
                        return t_e2

                    t_es = {0: emit_S(0)}
                    for i in range(32):
                        cp, h = items[i]
                        b = i % 2
                        ai = cp % 2
                        if i + 1 < 32:
                            t_es[i + 1] = emit_S(i + 1)
                        PE.wait(t_es[i], t_vd, t_va, pO_free[b])
                        for blk in range(5):
                            ins = nc.tensor.matmul(pO[b][:, 0:65], lhsT=pT[b][:, blk, :], rhs=va[s][:, cp + blk, h, :], start=(blk == 0), stop=(blk == 4))
                        tO = PE.done(ins)
                        pT_free[b] = tO
                        t_r = DVE.op(lambda: nc.vector.reciprocal(out=rinv[b][:], in_=pO[b][:, 64:65]), tO)
                        t_a = DVE.op(lambda: nc.vector.tensor_scalar(out=att[ai][:, h * 64:(h + 1) * 64], in0=pO[b][:, 0:64], scalar1=rinv[b][:, 0:1], scalar2=None, op0=ALU.mult), t_r, att_free[ai])
                        pO_free[b] = t_a
                        if h == 1:
                            PE.wait(t_a, pTr_free, pP_free[1])
                            tT = PE.done(nc.tensor.transpose(out=pTr[:, 0:128], in_=att[ai][:], identity=ident_bf[:]))
                            att_free[ai] = tT
                            last_pe = tT
                            pTr_free = ACT.op(lambda: nc.scalar.copy(out=catT[:, 8 + hp, cp * 128:(cp + 1) * 128], in_=pTr[:, 0:128]), tT)
                    wq_free[s] = last_pe
                    prev_att = last_pe
                    pP_free[1] = [pP_free[1], pTr_free]
                barrier()
        if stage == 1:
            s_d = Slot(nc, "dbg", st)
            t = s_d.dma(SP, lambda: nc.sync.dma_start(out=dbg_cat[:, :], in_=catT[:].rearrange("p a b -> p (a b)")))
            SP.wait(t)
            ph_cat.close()
            return nc


        with ExitStack() as ph:
            wo = sb("wo", [128, 16, D], BF16, ph)
            l1 = sb("l1", [128, 2, D], F32, ph)
            hld = [sb(f"hld{i}", [128, D], F32, ph) for i in range(3)]
            h1f = [sb(f"h1f{i}", [128, D], F32, ph) for i in range(2)]
            h1b = [sb(f"h1b{i}", [128, D], BF16, ph) for i in range(2)]
            lnp = [(sb(f"st4{i}", [128, 24], F32, ph), sb(f"mv4{i}", [128, 2], F32, ph), sb(f"sd4{i}", [128, 4], F32, ph)) for i in range(3)]
            pM = [pst(f"pM{i}", [128, 512], F32, ph) for i in range(8)]
            s_wo = Slot(nc, "wo", st)
            s_l1 = Slot(nc, "l1", st)
            s_hl = [Slot(nc, f"hl{i}", st) for i in range(3)]
            s_o1 = [Slot(nc, f"o1{i}", st) for i in range(2)]
            s_o2 = [Slot(nc, f"o2{i}", st) for i in range(2)]
            for dc in range(16):
                t_wo = s_wo.dma(POOL, lambda: nc.gpsimd.dma_start(out=wo[:, dc, :], in_=w_out[dc * 128:(dc + 1) * 128, :]))
            t_l1 = s_l1.dma(SP, lambda: nc.sync.dma_start(out=l1[:, 0, :], in_=ln12[0:1, :].partition_broadcast(128)))
            t_l1 = s_l1.dma(SP, lambda: nc.sync.dma_start(out=l1[:, 1, :], in_=ln12[1:2, :].partition_broadcast(128)))
            hld_free = [None] * 3
            h1f_free = [None, None]
            h1b_free = [None, None]
            pM_free = [None, None]

            def p4_A(tt):
                s = tt % 3
                ps = tt % 2
                rs = slice(tt * 128, (tt + 1) * 128)
                t_h = s_hl[s].dma(SP, lambda: nc.sync.dma_start(out=hld[s][:], in_=hbuf[rs, :]), hld_free[s])
                PE.wait(t_wo, pM_free[ps])
                for nb in range(4):
                    for ec in range(16):
                        ins = nc.tensor.matmul(pM[ps * 4 + nb][:, :], lhsT=catT[:, ec, rs], rhs=wo[:, ec, nb * 512:(nb + 1) * 512], start=(ec == 0), stop=(ec == 15))
                tM = PE.done(ins)
                DVE.wait(t_h, tM)
                for nb in range(4):
                    cs = slice(nb * 512, (nb + 1) * 512)
                    t = DVE.op(lambda: nc.vector.scalar_tensor_tensor(out=hld[s][:, cs], in0=hld[s][:, cs], scalar=ALPHA, in1=pM[ps * 4 + nb][:, :], op0=ALU.mult, op1=ALU.add))
                pM_free[ps] = t
                return t

            t_As = {0: p4_A(0)}
            for tt in range(16):
                s = tt % 3
                hs = tt % 2
                rs = slice(tt * 128, (tt + 1) * 128)
                t_n = ln_norm_act(hld[s], t_As[tt], lnp[s])
                if tt + 1 < 16:
                    t_As[tt + 1] = p4_A(tt + 1)
                t = DVE.op(lambda: nc.vector.tensor_tensor(out=hld[s][:], in0=hld[s][:], in1=l1[:, 0, :], op=ALU.mult), t_n, t_l1)
                t_f = POOL.op(lambda: nc.gpsimd.tensor_tensor(out=h1f[hs][:], in0=hld[s][:], in1=l1[:, 1, :], op=ALU.add), t, h1f_free[hs])
                hld_free[s] = t_f
                t_b16 = ACT.op(lambda: nc.scalar.copy(out=h1b[hs][:], in_=h1f[hs][:]), t_f, h1b_free[hs])
                t_s1 = s_o1[hs].dma(SP, lambda: nc.sync.dma_start(out=h1buf[rs, :], in_=h1f[hs][:]), t_f)
                t_s2 = s_o2[hs].dma(SP, lambda: nc.sync.dma_start(out=h1bf[rs, :], in_=h1b[hs][:]), t_b16)
                h1f_free[hs] = [t_b16, t_s1]
                h1b_free[hs] = t_s2
            barrier()
        ph_cat.close()
        if stage == 2:
            SP.wait([sl.tok() for sl in Slot.allslots])
            return nc

        G_all = sb("G_all", [128, 16, NE], F32)
        selm_bf = sb("selm_bf", [128, 16, NE], BF16)
        selm_f = sb("selm_f", [128, 16, NE], F32)
        slot8_all = sb("slot8_all", [128, 16, 8], I32)
        gate8_all = sb("gate8_all", [128, 16, 8], F32)
        with ExitStack() as ph_moe:
            wgb = [sb(f"wgb{i}", [128, 16, FH], BF16, ph_moe) for i in range(2)]
            wub = [sb(f"wub{i}", [128, 16, FH], BF16, ph_moe) for i in range(2)]
            wdb = [sb(f"wdb{i}", [128, 4, D], BF16, ph_moe) for i in range(2)]
            xg1 = sb("xg1", [128, 4, D], BF16, ph_moe)
            xg = [xg1, xg1]
            xT = [sb(f"xT{i}", [128, 16, CAP], BF16, ph_moe) for i in range(2)]
            hid = sb("hid", [128, 4, CAP], BF16, ph_moe)
            sg = [sb(f"sg{i}", [128, CAP], F32, ph_moe) for i in range(2)]
            yo = [sb(f"yo{i}", [128, D], BF16, ph_moe) for i in range(2)]
            pT2 = pst("pT2", [128, 1024], BF16, ph_moe)
            pGm2 = [pst(f"pGm{i}", [128, 512], F32, ph_moe) for i in range(2)]
            pUm2 = [pst(f"pUm{i}", [128, 512], F32, ph_moe) for i in range(2)]
            pYm2 = [pst(f"pYm{i}", [128, 512], F32, ph_moe) for i in range(2)]
            pYm = [pYm2[0], pYm2[1], pYm2[0], pYm2[1]]
            pR = pst("pR", [128, 512], F32, ph_moe)
            s_wgu = [Slot(nc, f"wgu{i}", st) for i in range(2)]
            s_wd = [Slot(nc, f"wd{i}", st) for i in range(2)]
            s_xg = [Slot(nc, f"xg{i}", st) for i in range(2)]
            s_yo = [Slot(nc, f"yo{i}", st) for i in range(2)]
            fst = dict(wgu_free=[None, None], wd_free=[None, None], xg_free=[None, None], xT_free=[None, None], pT2_free=[None, None], itp=0, tp_banks=[pT2[:, :]], pG_free=[None, None], pU_free=[None, None],
                       hid_free=None, sg_free=[None, None], pY_free=[None] * 4, yo_free=[None, None], isg=0, iyo=0)

            def load_w(s, wg_ap, wu_ap, wd_ap):
                t = s_wgu[s].dma(POOL, lambda: nc.gpsimd.dma_start(out=wgb[s][:].rearrange("p c f -> p (c f)"), in_=wg_ap.rearrange("(p c) f -> p (c f)", c=16), max_dma_last_dim=8192), fst["wgu_free"][s])
                t_gu = s_wgu[s].dma(POOL, lambda: nc.gpsimd.dma_start(out=wub[s][:].rearrange("p c f -> p (c f)"), in_=wu_ap.rearrange("(p c) f -> p (c f)", c=16), max_dma_last_dim=8192))
                t_d = s_wd[s].dma(POOL, lambda: nc.gpsimd.dma_start(out=wdb[s][:], in_=wd_ap.rearrange("(c p) n -> p c n", p=128)), fst["wd_free"][s])
                return t_gu, t_d

            def transpose_block(s, ntile, t_x, j0=0):
                for j in range(ntile):
                    for g in range(2):
                        banks = fst["tp_banks"]
                        bi = fst["itp"] % len(banks)
                        fst["itp"] += 1
                        pTb = banks[bi]
                        PE.wait(t_x, fst["pT2_free"][bi], fst["xT_free"][s])
                        for i in range(8):
                            dc = g * 8 + i
                            ins = nc.tensor.transpose(out=pTb[:, i * 128:(i + 1) * 128], in_=xg1[:, j0 + j, dc:D:16], identity=ident_bf[:])
                        tp = PE.done(ins)
                        src = pTb[:, :].rearrange("p (a b) -> p a b", a=8)
                        if g == 0:
                            t = ACT.op(lambda: nc.scalar.copy(out=xT[s][:, 0:8, j * 128:(j + 1) * 128], in_=src), tp, fst["xT_free"][s])
                        else:
                            t = DVE.op(lambda: nc.vector.tensor_copy(out=xT[s][:, 8:16, j * 128:(j + 1) * 128], in_=src), tp, fst["xT_free"][s])
                        fst["pT2_free"][bi] = t
                fst["xg_free"][s] = tp
                return [ACT.last, DVE.last]

            def ffn_gu(s, xs, W, t_w, t_xT):
                for fc in range(4):
                    pb = fc % 2
                    pGm = pGm2[pb]
                    pUm = pUm2[pb]
                    PE.wait(t_w, t_xT, fst["pG_free"][pb])
                    for dc in range(16):
                        ins = nc.tensor.matmul(pGm[:, 0:W], lhsT=wgb[s][:, dc, fc * 128:(fc + 1) * 128], rhs=xT[xs][:, dc, 0:W], start=(dc == 0), stop=(dc == 15))
                    tG = PE.done(ins)
                    PE.wait(fst["pU_free"][pb])
                    for dc in range(16):
                        ins = nc.tensor.matmul(pUm[:, 0:W], lhsT=wub[s][:, dc, fc * 128:(fc + 1) * 128], rhs=xT[xs][:, dc, 0:W], start=(dc == 0), stop=(dc == 15))
                    tU = PE.done(ins)
                    q = fst["isg"] % 2
                    fst["isg"] += 1
                    t_sg = ACT.op(lambda: nc.scalar.activation(out=sg[q][:, 0:W], in_=pGm[:, 0:W], func=AF.Silu), tG, fst["sg_free"][q])
                    fst["pG_free"][pb] = t_sg
                    t_h = DVE.op(lambda: nc.vector.tensor_tensor(out=hid[:, fc, 0:W], in0=pUm[:, 0:W], in1=sg[q][:, 0:W], op=ALU.mult), tU, t_sg, fst["hid_free"])
                    fst["pU_free"][pb] = t_h
                    fst["sg_free"][q] = t_h
                fst["xT_free"][xs] = tU
                fst["wgu_free"][s] = tU
                return t_h

            def ffn_d(s, W, t_wd, t_h, store):
                last = None
                for j in range((W + 127) // 128):
                    q = fst["iyo"] % 2
                    fst["iyo"] += 1
                    nr = min(128, W - j * 128)
                    tys = []
                    for nb in range(4):
                        PE.wait(t_h, t_wd, fst["pY_free"][nb % 2])
                        for fc in range(4):
                            ins = nc.tensor.matmul(pYm[nb][0:nr, :], lhsT=hid[:, fc, j * 128:j * 128 + nr], rhs=wdb[s][:, fc, nb * 512:(nb + 1) * 512], start=(fc == 0), stop=(fc == 3))
                        tY = PE.done(ins)
                        cs = slice(nb * 512, (nb + 1) * 512)
                        if nb % 2 == 0:
                            te = ACT.op(lambda: nc.scalar.copy(out=yo[q][0:nr, cs], in_=pYm[nb][0:nr, :]), tY, fst["yo_free"][q])
                        else:
                            te = DVE.op(lambda: nc.vector.tensor_copy(out=yo[q][0:nr, cs], in_=pYm[nb][0:nr, :]), tY, fst["yo_free"][q])
                        fst["pY_free"][nb % 2] = te
                        tys.append(te)
                        last = tY
                    fst["yo_free"][q] = store(j, yo[q], tys, s_yo[q])
                fst["hid_free"] = last
                fst["wd_free"][s] = last

            tokidx_all = sb("tokidx_all", [128, NE * NTL, 4], I32, ph_moe)
            with ExitStack() as ph:
                wr_b = sb("wr_b", [128, 16, NE], BF16, ph)
                rb_row = sb("rb_row", [128, NE], F32, ph)
                sc2 = sb("sc2", [128, 2, NE], F32, ph)
                selv = sb("selv", [128, 2, NE], F32, ph)
                m8 = sb("m8", [128, 2, 8, 8], F32, ph)
                gs = sb("gs", [128, 2, 8], F32, ph)
                gs8 = sb("gs8", [128, 2, 8], F32, ph)
                gm = sb("gm", [128, 2, 8], F32, ph)
                gm1 = sb("gm1", [128, 2, 8], F32, ph)
                msk = sb("msk", [128, 2, NE], F32, ph)
                t8 = sb("t8", [128, 2, 8], F32, ph)
                wv = sb("wv", [128, 2, NE], F32, ph)
                den = sb("den", [128, 2, 2], F32, ph)
                zt = sb("zt", [128, 1028], I32, ph)
                tokrow = sb("tokrow", [128, 16, 4], I32, ph)
                av = sb("av", [128, NE], F32, ph)
                vals = sb("vals", [128, NE], F32, ph)
                s8 = sb("s8", [128, 8], F32, ph)
                s8b = sb("s8b", [128, 8], F32, ph)
                e8 = sb("e8", [128, 8], F32, ph)
                junk = sb("junk", [128, NE], F32, ph)
                dump1 = sb("dump1", [128, 1], F32, ph)
                s_r = Slot(nc, "rt", st)
                s_r2 = Slot(nc, "rt2", st)
                s_z = Slot(nc, "z", st)
                s_sc = Slot(nc, "scat", st)
                s_ti = [Slot(nc, f"tix{i}", st) for i in range(8)]
                t_r = s_r.dma(POOL, lambda: nc.gpsimd.dma_start(out=wr_b[:], in_=w_router.rearrange("(p c) e -> p c e", c=16)))
                t_r = [t_r, s_r2.dma(SP, lambda: nc.sync.dma_start(out=rb_row[:], in_=router_bias[0:1, :].partition_broadcast(128)))]
                t = POOL.op(lambda: nc.gpsimd.memset(zt[:], 0))
                t = POOL.op(lambda: nc.gpsimd.memset(tokrow[:], 0), t)
                stz = slot_tok.rearrange("(p r) c -> p (r c)", p=128)
                t_z = s_z.dma(SP, lambda: nc.sync.dma_start(out=stz[:, :], in_=zt[:]), t)
                t_tr = DVE.op(lambda: nc.vector.tensor_copy(out=tokrow[:, :, 0], in_=cst[:, 512:528]), t)
                t_d1 = DVE.op(lambda: nc.vector.tensor_scalar(out=dump1[:], in0=cst[:, 528:529], scalar1=1.0, scalar2=None, op0=ALU.add))
                t_wgu, t_wd = load_w(0, wsg, wsu, wsd)
                t_ws = {}
                if stage >= 5:
                    t_ws[0] = load_w(1, w_gate[0], w_up[0], w_down[0])

                def lvl(fns, *deps):
                    DVE.wait(*deps)
                    t = None
                    for fn in fns:
                        t = DVE.done(fn())
                    return t

                t_pR_free = None
                t_sc = None
                for tb in range(8):
                    s = tb % 2
                    g0 = tb * 2
                    t_x = s_xg[s].dma(SP, lambda: nc.sync.dma_start(out=xg1[:, 2 * s:2 * s + 2, :], in_=h1bf[tb * 256:(tb + 1) * 256, :].rearrange("(j p) d -> p j d", p=128)), fst["xg_free"][s])
                    t_xT = transpose_block(s, 2, t_x, j0=2 * s)
                    PE.wait(t_xT, t_r, t_pR_free)
                    for j in range(2):
                        for dc in range(16):
                            ins = nc.tensor.matmul(pR[:, j * NE:(j + 1) * NE], lhsT=xT[s][:, dc, j * 128:(j + 1) * 128], rhs=wr_b[:, dc, :], start=(dc == 0), stop=(dc == 15))
                    tR = PE.done(ins)
                    t_sig = ACT.op(lambda: nc.scalar.activation(out=sc2[:].rearrange("p a b -> p (a b)"), in_=pR[:, 0:2 * NE], func=AF.Sigmoid), tR, t_pR_free)
                    t = lvl([lambda j=j: nc.vector.tensor_tensor(out=selv[:, j, :], in0=sc2[:, j, :], in1=rb_row[:], op=ALU.add) for j in range(2)], t_sig, t_r)
                    t = lvl([lambda j=j, g=g: nc.vector.max(out=m8[:, j, g, :], in_=selv[:, j, g * 8:(g + 1) * 8]) for j in range(2) for g in range(8)], t)
                    t = lvl([lambda: nc.vector.tensor_tensor(out=gs[:], in0=m8[:, :, :, 0], in1=m8[:, :, :, 1], op=ALU.add)], t)
                    t = lvl([lambda j=j: nc.vector.max(out=gs8[:, j, :], in_=gs[:, j, :]) for j in range(2)], t)
                    t = lvl([lambda j=j: nc.vector.tensor_scalar(out=gm[:, j, :], in0=gs[:, j, :], scalar1=gs8[:, j, 3:4], scalar2=None, op0=ALU.is_ge) for j in range(2)], t)
                    t = lvl([lambda: nc.vector.tensor_scalar(out=gm1[:], in0=gm[:], scalar1=-1.0, scalar2=None, op0=ALU.add)], t)
                    t = lvl([lambda j=j, g=g: nc.vector.tensor_scalar(out=msk[:, j, g * 8:(g + 1) * 8], in0=selv[:, j, g * 8:(g + 1) * 8], scalar1=gm[:, j, g:g + 1], scalar2=gm1[:, j, g:g + 1], op0=ALU.mult, op1=ALU.add) for j in range(2) for g in range(8)], t)
                    t = lvl([lambda j=j: nc.vector.max(out=t8[:, j, :], in_=msk[:, j, :]) for j in range(2)], t)
                    t = lvl([lambda j=j: nc.vector.tensor_scalar(out=selm_f[:, g0 + j, :], in0=msk[:, j, :], scalar1=t8[:, j, 7:8], scalar2=None, op0=ALU.is_ge) for j in range(2)], t)
                    t_selm = lvl([lambda: nc.vector.tensor_copy(out=selm_bf[:, g0:g0 + 2, :], in_=selm_f[:, g0:g0 + 2, :]),
                                  lambda: nc.vector.tensor_tensor(out=wv[:], in0=sc2[:], in1=selm_f[:, g0:g0 + 2, :], op=ALU.mult)], t)
                    t = lvl([lambda: nc.vector.tensor_reduce(out=den[:, 0, :], in_=wv[:], axis=mybir.AxisListType.X, op=ALU.add)], t_selm)
                    t = lvl([lambda: nc.vector.reciprocal(out=den[:, 1, :], in_=den[:, 0, :])], t)
                    t_G = lvl([lambda j=j: nc.vector.tensor_scalar(out=G_all[:, g0 + j, :], in0=wv[:, j, :], scalar1=den[:, 1, j:j + 1], scalar2=2.5, op0=ALU.mult, op1=ALU.mult) for j in range(2)], t)

                    def store_sh(j, yot, tys, slot, tb=tb):
                        r0 = (tb * 2 + j) * 128
                        return slot.dma(SP, lambda: nc.sync.dma_start(out=yshbuf[r0:r0 + 128, :], in_=yot[:]), tys)
                    t_h = ffn_gu(0, s, 256, t_wgu, t_xT)
                    ffn_d(0, 256, t_wd, t_h, store_sh)
                    PE.wait(t_selm, t_sig)
                    for j in range(2):
                        tt = g0 + j
                        o = pR[:, 128 + j * NE:128 + (j + 1) * NE]
                        ins = nc.tensor.matmul(o, lhsT=lstr_bf[:], rhs=selm_bf[:, tt, :], start=True, stop=(tt == 0))
                        for i in range(tt):
                            ins = nc.tensor.matmul(o, lhsT=ones_bf[:], rhs=selm_bf[:, i, :], start=False, stop=(i == tt - 1))
                    tP = PE.done(ins)
                    t = t_G
                    for j in range(2):
                        tt = g0 + j
                        pos = pR[:, 128 + j * NE:128 + (j + 1) * NE]
                        t = DVE.op(lambda: nc.vector.scalar_tensor_tensor(out=av[:], in0=pos, scalar=float(CAPV), in1=selm_f[:, tt, :], op0=ALU.is_lt, op1=ALU.mult), tP, t)
                        t = DVE.op(lambda: nc.vector.tensor_tensor(out=vals[:], in0=pos, in1=cst[:, 529:593], op=ALU.add), t)
                        t_pR_free = t
                        t = DVE.op(lambda: nc.vector.tensor_tensor(out=vals[:], in0=vals[:], in1=av[:], op=ALU.mult), t)
                        t = DVE.op(lambda: nc.vector.tensor_scalar(out=vals[:], in0=vals[:], scalar1=-1.0, scalar2=None, op0=ALU.add), t)
                        t = DVE.op(lambda: nc.vector.max(out=s8[:], in_=vals[:]), t)
                        t = DVE.op(lambda: nc.vector.tensor_scalar(out=s8b[:], in0=s8[:], scalar1=0.0, scalar2=dump1[:, 0:1], op0=ALU.is_lt, op1=ALU.mult), t, t_d1)
                        t = DVE.op(lambda: nc.vector.tensor_tensor(out=s8b[:], in0=s8b[:], in1=s8[:], op=ALU.add), t)
                        t_s8 = DVE.op(lambda: nc.vector.tensor_copy(out=slot8_all[:, tt, :], in_=s8b[:]), t)
                        t = DVE.op(lambda: nc.vector.scalar_tensor_tensor(out=vals[:], in0=cst[:, 384:448], scalar=1.0, in1=av[:], op0=ALU.add, op1=ALU.mult), t_s8)
                        t = DVE.op(lambda: nc.vector.tensor_scalar(out=vals[:], in0=vals[:], scalar1=-1.0, scalar2=None, op0=ALU.add), t)
                        t = DVE.op(lambda: nc.vector.max(out=e8[:], in_=vals[:]), t)
                        for k in range(8):
                            t = DVE.op(lambda: nc.vector.scalar_tensor_tensor(out=junk[:], in0=cst[:, 384:448], scalar=e8[:, k:k + 1], in1=G_all[:, tt, :], op0=ALU.is_equal, op1=ALU.mult, accum_out=gate8_all[:, tt, k:k + 1]), t)
                        for k in range(8):
                            t_sc = s_sc.dma(POOL, lambda: nc.gpsimd.indirect_dma_start(out=slot_tok[:, :], out_offset=bass.IndirectOffsetOnAxis(ap=slot8_all[:, tt, k:k + 1], axis=0), in_=tokrow[:, tt, :], in_offset=None), t_s8, t_z, t_tr)
                if stage >= 5:
                    t_ws[1] = load_w(0, w_gate[1], w_up[1], w_down[1])
                stv = slot_tok[0:NS, :].rearrange("(r p) c -> p r c", p=128)
                t_tis = []
                for i in range(8):
                    t_tis.append(s_ti[i].dma(SP, lambda: nc.sync.dma_start(out=tokidx_all[:, i * 32:(i + 1) * 32, :], in_=stv[:, i * 32:(i + 1) * 32, :]), t_sc))
                barrier(exclude=s_wgu + s_wd + (s_ti if stage >= 5 else []))
            if stage in (3, 4):
                s_d = Slot(nc, "dbg", st)
                t = s_d.dma(SP, lambda: nc.sync.dma_start(out=dbg_gate[:, :], in_=G_all[:].rearrange("p a b -> p (a b)")))
                if stage == 4:
                    t = s_d.dma(SP, lambda: nc.sync.dma_start(out=dbg_s8[:, :], in_=slot8_all[:].rearrange("p a b -> p (a b)")))
                SP.wait(t, [sl.tok() for sl in Slot.allslots])
                return nc

            s_xgp = [Slot(nc, f"xgp{i}", st) for i in range(2)]
            fst["tp_banks"] = [pT2[:, :], pR[:, :].bitcast(BF16)]
            fst["itp"] = 0

            def gather_e(e):
                s = e % 2
                t_x = None
                for m in range(NTL):
                    t_x = s_xgp[s].dma(POOL, lambda: nc.gpsimd.indirect_dma_start(out=xg1[:, m, :], out_offset=None, in_=h1bf[:, :], in_offset=bass.IndirectOffsetOnAxis(ap=tokidx_all[:, e * NTL + m, 0:1], axis=0)), (fst["xg_free"][0], fst["xg_free"][1], t_tis[e // 8]) if m == 0 else None)
                return t_x

            t_xg = {0: gather_e(0)}
            t_xTs = {0: transpose_block(0, NTL, t_xg[0])}
            for e in range(NE):
                s = e % 2
                ws = (e + 1) % 2
                if e + 1 < NE:
                    t_xg[e + 1] = gather_e(e + 1)
                    if e + 1 >= 2:
                        t_ws[e + 1] = load_w((e + 2) % 2, w_gate[e + 1], w_up[e + 1], w_down[e + 1])
                t_h = ffn_gu(ws, s, CAPV, t_ws[e][0], t_xTs[e])
                if e + 1 < NE:
                    t_xTs[e + 1] = transpose_block((e + 1) % 2, NTL, t_xg[e + 1])

                def store_e(j, yot, tys, slot, e=e):
                    r0 = e * CAP + j * 128
                    return slot.dma(SP, lambda: nc.sync.dma_start(out=ybuf[r0:r0 + 128, :], in_=yot[:, :]), tys)
                ffn_d(ws, CAPV, t_ws[e][1], t_h, store_e)
            barrier()

        with ExitStack() as ph:
            l2 = sb("l2", [128, 2, D], F32, ph)
            acc = [sb(f"acc{i}", [128, D], F32, ph) for i in range(4)]
            ysh = [sb(f"ysh{i}", [128, D], BF16, ph) for i in range(3)]
            yg = [sb(f"yg{i}", [128, D], BF16, ph) for i in range(24)]
            zf = sb("zf", [128, D], BF16, ph)
            dgt = [sb(f"dgt{i}", [128, 16, 128], BF16, ph) for i in range(3)]
            ghl = [sb(f"ghl{i}", [128, 2, 8], F32, ph) for i in range(3)]
            ghb = [sb(f"ghb{i}", [128, 8], BF16, ph) for i in range(3)]
            pC = [pst(f"pC{i}", [128, 512], F32, ph) for i in range(8)]
            lnp = [(sb(f"st7{i}", [128, 24], F32, ph), sb(f"mv7{i}", [128, 2], F32, ph), sb(f"sd7{i}", [128, 4], F32, ph)) for i in range(4)]
            s_l2 = Slot(nc, "l2", st)
            s_zf = Slot(nc, "zf", st)
            s_a = [Slot(nc, f"a{i}", st) for i in range(4)]
            s_ys = [Slot(nc, f"ys{i}", st) for i in range(3)]
            s_yg = [Slot(nc, f"yg{i}", st) for i in range(24)]
            s_out = [Slot(nc, f"out{i}", st) for i in range(4)]
            t_l2 = s_l2.dma(SP, lambda: nc.sync.dma_start(out=l2[:, 0, :], in_=ln12[2:3, :].partition_broadcast(128)))
            t_l2 = s_l2.dma(SP, lambda: nc.sync.dma_start(out=l2[:, 1, :], in_=ln12[3:4, :].partition_broadcast(128)))
            t = POOL.op(lambda: nc.gpsimd.memset(zf[:], 0.0))
            t_zf = s_zf.dma(SP, lambda: nc.sync.dma_start(out=ybuf[NS:NS + 128, :], in_=zf[:]), t)
            acc_free = [None] * 4
            ysh_free = [None] * 3
            yg_free = [None] * 24
            dgt_free = [None] * 3
            pC_free = [None, None]
            tk = {}

            def p7_A0(tt):
                s = tt % 4
                ys = tt % 3
                rs = slice(tt * 128, (tt + 1) * 128)
                t_a = s_a[s].dma(SP, lambda: nc.sync.dma_start(out=acc[s][:], in_=h1buf[rs, :]), acc_free[s])
                t_y = s_ys[ys].dma(SP, lambda: nc.sync.dma_start(out=ysh[ys][:], in_=yshbuf[rs, :]), ysh_free[ys])
                t_gs = []
                for k in range(8):
                    q = ys * 8 + k
                    t_gs.append(s_yg[q].dma(POOL, lambda: nc.gpsimd.indirect_dma_start(out=yg[q][:], out_offset=None, in_=ybuf[:, :], in_offset=bass.IndirectOffsetOnAxis(ap=slot8_all[:, tt, k:k + 1], axis=0)), yg_free[q], t_zf))
                t = DVE.op(lambda: nc.vector.tensor_copy(out=ghb[ys][:], in_=gate8_all[:, tt, :]), dgt_free[ys])
                t = DVE.op(lambda: nc.vector.tensor_copy(out=ghl[ys][:, 0, :], in_=ghb[ys][:]), t)
                t = DVE.op(lambda: nc.vector.tensor_tensor(out=ghl[ys][:, 1, :], in0=gate8_all[:, tt, :], in1=ghl[ys][:, 0, :], op=ALU.subtract), t)
                ACT.wait(t, dgt_free[ys])
                for i in range(16):
                    t_dg = ACT.done(nc.scalar.activation(out=dgt[ys][:, i, :], in_=ident_bf[:], func=AF.Copy, scale=ghl[ys][:, i // 8, i % 8:i % 8 + 1]))
                t_base = DVE.op(lambda: nc.vector.scalar_tensor_tensor(out=acc[s][:], in0=acc[s][:], scalar=ALPHA, in1=ysh[ys][:], op0=ALU.mult, op1=ALU.add), t_a, t_y)
                ysh_free[ys] = t_base
                tk[tt] = (t_gs, t_dg, t_base)

            def p7_A1(tt):
                s = tt % 4
                ys = tt % 3
                ps = tt % 2
                t_gs, t_dg, t_base = tk[tt]
                PE.wait(t_dg, pC_free[ps])
                for k in range(8):
                    PE.wait(t_gs[k])
                    for hl in range(2):
                        for nb in range(4):
                            ins = nc.tensor.matmul(pC[ps * 4 + nb][:, :], lhsT=dgt[ys][:, hl * 8 + k, :], rhs=yg[ys * 8 + k][:, nb * 512:(nb + 1) * 512], start=(k == 0 and hl == 0), stop=(k == 7 and hl == 1))
                tC = PE.done(ins)
                for k in range(8):
                    yg_free[ys * 8 + k] = tC
                dgt_free[ys] = tC
                DVE.wait(tC, t_base)
                for nb in range(4):
                    cs = slice(nb * 512, (nb + 1) * 512)
                    t = DVE.done(nc.vector.tensor_tensor(out=acc[s][:, cs], in0=acc[s][:, cs], in1=pC[ps * 4 + nb][:, :], op=ALU.add))
                pC_free[ps] = t
                return t

            p7_A0(0)
            p7_A0(1)
            t_As = {0: p7_A1(0)}
            for tt in range(16):
                s = tt % 4
                rs = slice(tt * 128, (tt + 1) * 128)
                if tt + 2 < 16:
                    p7_A0(tt + 2)
                t_n = ln_norm_act(acc[s], t_As[tt], lnp[s])
                t = DVE.op(lambda: nc.vector.tensor_tensor(out=acc[s][:], in0=acc[s][:], in1=l2[:, 0, :], op=ALU.mult), t_n, t_l2)
                t = DVE.op(lambda: nc.vector.tensor_tensor(out=acc[s][:], in0=acc[s][:], in1=l2[:, 1, :], op=ALU.add), t)
                acc_free[s] = s_out[s].dma(ACT, lambda: nc.scalar.dma_start(out=y[rs, :], in_=acc[s][:]), t)
                if tt + 1 < 16:
                    t_As[tt + 1] = p7_A1(tt + 1)
            SP.wait([sl.tok() for sl in Slot.allslots])
    return nc


def prep_inputs(inp, stage=99):
    f = np.float32
    x = np.asarray(inp["x"], f)
    common = {}
    cst = np.zeros((128, NCONST), f)
    cst[:, 0:128] = np.eye(128, dtype=f)
    cst[:, 128:256] = np.triu(np.ones((128, 128), f), 1)
    cst[:, 256:384] = 1.0
    cst[:, 384:448] = np.arange(64, dtype=f)[None, :]
    cst[:, 448:512] = (np.arange(64, dtype=f) * CAP)[None, :]
    cst[:, 512:528] = np.arange(16, dtype=f)[None, :] * 128 + np.arange(128, dtype=f)[:, None]
    cst[:, 528] = NS + np.arange(128, dtype=f)
    cst[:, 529:529 + 64] = np.arange(64, dtype=f)[None, :] * CAP + 1.0
    common["consts"] = cst
    common["ln_in_gb"] = np.stack([np.asarray(inp["ln_in_g"], f), np.asarray(inp["ln_in_b"], f)])
    common["w_in"] = np.ascontiguousarray(np.asarray(inp["w_in"], f)[0])
    cw = np.asarray(inp["conv_w"], f)[0]
    common["conv_wT"] = np.ascontiguousarray(cw.T.reshape(8, 128, 31).transpose(1, 0, 2).reshape(128, 8 * 31))
    cols = lambda v: np.asarray(v, f)[0].reshape(8, 128).T
    common["conv_par"] = np.ascontiguousarray(np.concatenate([cols(inp["conv_b"]), cols(inp["conv_ln_g"]), cols(inp["conv_ln_b"])], axis=1))
    rb = np.asarray(inp["rel_bias"], f)[0]
    qi = np.arange(128)
    kk = np.arange(640)
    rel = 512 + qi[None, :] - kk[:, None]
    idx = np.clip(rel, -63, 256) + 63
    bias = rb[:, idx]
    kloc = kk[:, None] - 64 * (qi[None, :] >= 64)
    valid = (kloc >= 0) & (kloc < 576)
    bias = np.where(valid[None], bias, f(-30000.0)).astype(f)
    common["biasT"] = np.ascontiguousarray(bias.reshape(16, 5, 128, 128).transpose(2, 0, 1, 3).reshape(128, 16 * 5 * 128))
    common["w_out"] = np.ascontiguousarray(np.asarray(inp["w_out"], f)[0])
    common["ln12"] = np.stack([np.asarray(inp[k], f)[0] for k in ("ln1_g", "ln1_b", "ln2_g", "ln2_b")])
    common["w_router"] = np.ascontiguousarray(np.asarray(inp["w_router"], f)[0])
    common["router_bias"] = np.asarray(inp["router_bias"], f).reshape(1, NE)
    if stage >= 5:
        common["w_gate"] = np.asarray(inp["w_gate"], f)[0]
        common["w_up"] = np.asarray(inp["w_up"], f)[0]
        common["w_down"] = np.asarray(inp["w_down"], f)[0]
    common["wsg"] = np.asarray(inp["w_shared_gate"], f)[0]
    common["wsu"] = np.asarray(inp["w_shared_up"], f)[0]
    common["wsd"] = np.asarray(inp["w_shared_down"], f)[0]
    maps = []
    for c in range(8):
        b, half = c // 2, c % 2
        xs = np.zeros((NTH, D), f)
        if half == 1:
            xs[:] = x[b, NT - HALO:2 * NT]
        else:
            xs[HALO:] = x[b, 0:NT]
        m = dict(common)
        m["x"] = xs
        m["flags"] = np.full((128, 2), float(half), f)
        maps.append(m)
    return maps


_NC_CACHE = {}


def kernel(**inputs):
    maps = prep_inputs(inputs)
    if "nc" not in _NC_CACHE:
        _NC_CACHE["nc"] = build_nc()
    res = run_bass_kernel_spmd(_NC_CACHE["nc"], maps, core_ids=list(range(8)))
    out = np.zeros((4, 2 * NT, D), np.float32)
    for c in range(8):
        b, half = c // 2, c % 2
        out[b, half * NT:(half + 1) * NT] = res.results[c]["y"]
    return out
```
